# Optimizing a Trainium2 kernel written in Bass

```python
import math
import jax, jax.numpy as jnp
from jax import lax
import numpy as np

D_MODEL = 1024
BATCH = 8
SEQ = 4096
DEPTH = 2

HEAD_DIM = 64
A_HEADS = 6
A_NOPE = 64
A_ROPE = 32
A_V = 64
Q_LORA = 768
KV_LORA = 256
ROPE_THETA = 10000.0
B_HEADS = 6
DILATED_PATTERNS = ((128, 1), (512, 4), (2048, 16))
C_HEADS = 4
IDX_HEADS = 8
IDX_DIM = 64
TOPK_MAX = 256
Q_BLOCK = 128

A_WIDTH = A_HEADS * A_V
B_WIDTH = B_HEADS * HEAD_DIM
C_WIDTH = C_HEADS * HEAD_DIM
MIX_WIDTH = A_WIDTH + B_WIDTH + C_WIDTH
N_ALIBI = B_HEADS + C_HEADS

SPLIT_SIZES = (Q_LORA, KV_LORA, A_ROPE,
               B_WIDTH, B_WIDTH, B_WIDTH,
               C_WIDTH, C_WIDTH, C_WIDTH,
               IDX_HEADS * IDX_DIM, IDX_DIM, IDX_HEADS,
               MIX_WIDTH)
IN_WIDTH = sum(SPLIT_SIZES)
SPLIT_POINTS = [int(v) for v in np.cumsum(SPLIT_SIZES)[:-1]]

ALPHA = (2 * DEPTH) ** 0.25
BETA = (8 * DEPTH) ** -0.25

kernel_name = "hybrid_mla_dilated_dsa_deepnorm"

F32 = jnp.float32


def layer_norm(x, g, b, eps=1e-5):
    xf = x.astype(F32)
    mu = jnp.mean(xf, axis=-1, keepdims=True)
    xc = xf - mu
    var = jnp.mean(xc * xc, axis=-1, keepdims=True)
    return (xc * lax.rsqrt(var + eps) * g.astype(F32) + b.astype(F32)).astype(x.dtype)


def rms_norm(x, g, eps=1e-6):
    xf = x.astype(F32)
    ms = jnp.mean(xf * xf, axis=-1, keepdims=True)
    return (xf * lax.rsqrt(ms + eps) * g.astype(F32)).astype(x.dtype)


def apply_rope(x, cos, sin):
    half = x.shape[-1] // 2
    x1, x2 = x[..., :half].astype(F32), x[..., half:].astype(F32)
    return jnp.concatenate([x1 * cos - x2 * sin, x1 * sin + x2 * cos], axis=-1).astype(x.dtype)


def alibi_slopes():
    return 2.0 ** (-8.0 * jnp.arange(1, N_ALIBI + 1, dtype=F32) / N_ALIBI)


def to_blocks(a):
    b, s = a.shape[0], a.shape[1]
    return a.reshape((b, s // Q_BLOCK, Q_BLOCK) + a.shape[2:]).swapaxes(0, 1)


def from_blocks(a):
    a = a.swapaxes(0, 1)
    return a.reshape((a.shape[0], a.shape[1] * a.shape[2]) + a.shape[3:])


def mla_attention(q_nope, q_rope, k_nope, k_rope, v):
    s_len = q_nope.shape[1]
    nb = s_len // Q_BLOCK
    scale = (A_NOPE + A_ROPE) ** -0.5
    key_pos = jnp.arange(s_len)

    def block(args):
        qn, qr, start = args
        qpos = start + jnp.arange(Q_BLOCK)
        s = (jnp.einsum('bqhd,bshd->bhqs', qn, k_nope, preferred_element_type=F32)
             + jnp.einsum('bqhr,bsr->bhqs', qr, k_rope, preferred_element_type=F32)) * scale
        s = jnp.where(key_pos[None, :] <= qpos[:, None], s, -jnp.inf)
        p = jax.nn.softmax(s, axis=-1)
        return jnp.einsum('bhqs,bshd->bqhd', p.astype(v.dtype), v)

    out = lax.map(block, (to_blocks(q_nope), to_blocks(q_rope), jnp.arange(nb) * Q_BLOCK))
    return from_blocks(out)


def dilated_attention(q, k, v, slopes):
    bsz, s_len, nh, dh = q.shape
    outs, lses = [], []
    for window, dil in DILATED_PATTERNS:
        n = window // dil
        L = s_len // dil
        nb = -(-L // n)
        Lp = nb * n

        def gather(a):
            a = a.reshape(bsz, L, dil, nh, dh).swapaxes(1, 2)
            a = jnp.pad(a, ((0, 0), (0, 0), (0, Lp - L), (0, 0), (0, 0)))
            return a.reshape(bsz, dil, nb, n, nh, dh)

        def band(a):
            prev = jnp.pad(a, ((0, 0), (0, 0), (1, 0), (0, 0), (0, 0), (0, 0)))[:, :, :-1]
            return jnp.concatenate([prev, a], axis=3)

        qb = gather(q)
        kb = band(gather(k))
        vb = band(gather(v))
        s = jnp.einsum('brnqhd,brnkhd->brnhqk', qb, kb, preferred_element_type=F32) * dh ** -0.5
        kj = jnp.arange(2 * n)
        step = jnp.arange(n)[:, None] + n - kj[None, :]
        valid = ((step >= 0) & (step <= n))[None] & ((jnp.arange(nb)[:, None, None] > 0) | (kj >= n)[None, None, :])
        s = s - slopes[:, None, None] * (step * dil).astype(F32)
        s = jnp.where(valid[:, None], s, -jnp.inf)
        m = jnp.max(s, axis=-1, keepdims=True)
        p = jnp.exp(s - m)
        den = jnp.sum(p, axis=-1, keepdims=True)
        o = jnp.einsum('brnhqk,brnkhd->brnqhd', (p / den).astype(v.dtype), vb)
        lse = (m + jnp.log(den))[..., 0].swapaxes(-1, -2)

        def ungather(a):
            tr = a.shape[4:]
            a = a.reshape((bsz, dil, Lp) + tr)[:, :, :L]
            return a.swapaxes(1, 2).reshape((bsz, s_len) + tr)

        outs.append(ungather(o))
        lses.append(ungather(lse))
    w = jax.nn.softmax(jnp.stack(lses, axis=0), axis=0)
    out = jnp.sum(w[..., None] * jnp.stack(outs, axis=0).astype(F32), axis=0)
    return out.astype(q.dtype)


def sparse_attention(q, k, v, q_idx, k_idx, w_idx, slopes):
    s_len, dh = q.shape[1], q.shape[3]
    topk = min(TOPK_MAX, s_len // 4)
    nb = s_len // Q_BLOCK
    key_pos = jnp.arange(s_len)
    take = jax.vmap(lambda a, i: a[i])

    def block(args):
        qb, qib, wb, start = args
        qpos = start + jnp.arange(Q_BLOCK)
        logits = jnp.einsum('bqhd,bsd->bqhs', qib, k_idx, preferred_element_type=F32) * IDX_DIM ** -0.5
        score = jnp.einsum('bqh,bqhs->bqs', wb.astype(F32) * IDX_HEADS ** -0.5, jax.nn.relu(logits))
        score = jnp.where(key_pos[None, None, :] <= qpos[None, :, None], score, -jnp.inf)
        _, idx = lax.top_k(score, topk)
        valid = idx <= qpos[None, :, None]
        kg = take(k, idx)
        vg = take(v, idx)
        s = jnp.einsum('bqhd,bqkhd->bhqk', qb, kg, preferred_element_type=F32) * dh ** -0.5
        dist = (qpos[None, :, None] - idx).astype(F32)
        s = s - slopes[None, :, None, None] * dist[:, None]
        s = jnp.where(valid[:, None], s, -jnp.inf)
        p = jax.nn.softmax(s, axis=-1)
        return jnp.einsum('bhqk,bqkhd->bqhd', p.astype(v.dtype), vg)

    out = lax.map(block, (to_blocks(q), to_blocks(q_idx), to_blocks(w_idx), jnp.arange(nb) * Q_BLOCK))
    return from_blocks(out)


def setup_inputs(seed: int = 0) -> dict:
    key = jax.random.key(seed)
    ks = jax.random.split(key, 14)
    nrm = jax.random.normal
    return {
        "x": nrm(ks[0], (BATCH, SEQ, D_MODEL), F32),
        "c": nrm(ks[1], (BATCH, D_MODEL), F32),
        "w_ada": nrm(ks[2], (DEPTH, D_MODEL, 3 * D_MODEL), F32) * (0.1 * D_MODEL ** -0.5),
        "b_ada": nrm(ks[3], (DEPTH, 3 * D_MODEL), F32) * 0.02,
        "w_in": nrm(ks[4], (DEPTH, D_MODEL, IN_WIDTH), F32) * D_MODEL ** -0.5,
        "q_norm_g": 1.0 + 0.02 * nrm(ks[5], (DEPTH, Q_LORA), F32),
        "kv_norm_g": 1.0 + 0.02 * nrm(ks[6], (DEPTH, KV_LORA), F32),
        "w_uq": nrm(ks[7], (DEPTH, Q_LORA, A_HEADS * (A_NOPE + A_ROPE)), F32) * Q_LORA ** -0.5,
        "w_uk": nrm(ks[8], (DEPTH, KV_LORA, A_HEADS * A_NOPE), F32) * KV_LORA ** -0.5,
        "w_uv": nrm(ks[9], (DEPTH, KV_LORA, A_HEADS * A_V), F32) * KV_LORA ** -0.5,
        "w_out": nrm(ks[10], (DEPTH, MIX_WIDTH, D_MODEL), F32) * (BETA * MIX_WIDTH ** -0.5),
        "ln_g": 1.0 + 0.02 * nrm(ks[11], (DEPTH, D_MODEL), F32),
        "ln_b": 0.02 * nrm(ks[12], (DEPTH, D_MODEL), F32),
    }


def reference(x, c, w_ada, b_ada, w_in, q_norm_g, kv_norm_g, w_uq, w_uk, w_uv, w_out, ln_g, ln_b):
    bsz, s_len, _ = x.shape
    slopes = alibi_slopes()
    slopes_b, slopes_c = slopes[:B_HEADS], slopes[B_HEADS:]
    pos = jnp.arange(s_len, dtype=F32)
    freqs = ROPE_THETA ** (-jnp.arange(0, A_ROPE, 2, dtype=F32) / A_ROPE)
    ang = pos[:, None] * freqs[None, :]
    cos, sin = jnp.cos(ang), jnp.sin(ang)

    def heads(a, n):
        return a.reshape(bsz, s_len, n, -1)

    for l in range(DEPTH):
        mod = jax.nn.silu(c) @ w_ada[l] + b_ada[l]
        shift, scale, gate = jnp.split(mod, 3, axis=-1)
        h = x * (1.0 + scale[:, None, :]) + shift[:, None, :]
        (p_cq, p_ckv, p_kr, p_qb, p_kb, p_vb, p_qc, p_kc, p_vc,
         p_qi, p_ki, p_wi, p_gate) = jnp.split(h @ w_in[l], SPLIT_POINTS, axis=-1)

        cq = rms_norm(p_cq, q_norm_g[l])
        ckv = rms_norm(p_ckv, kv_norm_g[l])
        qa = heads(cq @ w_uq[l], A_HEADS)
        qa_nope = qa[..., :A_NOPE]
        qa_rope = apply_rope(qa[..., A_NOPE:], cos[:, None, :], sin[:, None, :])
        ka_nope = heads(ckv @ w_uk[l], A_HEADS)
        va = heads(ckv @ w_uv[l], A_HEADS)
        ka_rope = apply_rope(p_kr, cos, sin)
        o_a = mla_attention(qa_nope, qa_rope, ka_nope, ka_rope, va)

        o_b = dilated_attention(heads(p_qb, B_HEADS), heads(p_kb, B_HEADS), heads(p_vb, B_HEADS), slopes_b)

        o_c = sparse_attention(heads(p_qc, C_HEADS), heads(p_kc, C_HEADS), heads(p_vc, C_HEADS),
                               heads(p_qi, IDX_HEADS), p_ki, p_wi, slopes_c)

        y = jnp.concatenate([o_a.reshape(bsz, s_len, A_WIDTH),
                             o_b.reshape(bsz, s_len, B_WIDTH),
                             o_c.reshape(bsz, s_len, C_WIDTH)], axis=-1) * jax.nn.silu(p_gate)
        sub = y @ w_out[l]
        x = layer_norm(ALPHA * x + (1.0 + gate[:, None, :]) * sub, ln_g[l], ln_b[l])
    return x
```

```python
import math
from contextlib import ExitStack
import numpy as np
import ml_dtypes
import concourse.bass as bass
import concourse.mybir as mybir
from concourse.bass_utils import run_bass_kernel_spmd

F32 = mybir.dt.float32
BF16 = mybir.dt.bfloat16
AF = mybir.ActivationFunctionType
ALU = mybir.AluOpType
AX = mybir.AxisListType

ENGS = ("pe", "act", "dve", "pool", "sp")
DMA_RING = {"sp": 40, "pool": 24, "act": 16}
SIG_CHUNK = 16384


class Buf:
    _n = 0

    def __init__(self, name="b"):
        Buf._n += 1
        self.name = f"{name}#{Buf._n}"

    def __getitem__(self, key):
        return (self, key)


def _norm(x):
    if isinstance(x, Buf):
        return (x, None)
    if isinstance(x, Tile):
        return (x.buf, None)
    return x


class Tile:
    def __init__(self, ap, name="t"):
        self.ap = ap
        self.buf = Buf(name)

    def __getitem__(self, key):
        return self.ap[key]

    def k(self, key):
        return (self.buf, key)


class Op:
    __slots__ = ("eng", "fn", "reads", "writes", "dma", "idx", "deps", "sig", "sigidx",
                 "dsem", "dval", "dprev", "barrier", "cons")

    def __init__(self, eng, fn, reads, writes, dma):
        self.eng = eng
        self.fn = fn
        self.reads = [_norm(r) for r in reads]
        self.writes = [_norm(w) for w in writes]
        self.dma = dma
        self.deps = []
        self.sig = False
        self.sigidx = None
        self.barrier = False
        self.cons = None


class Sched:
    def __init__(self, nc):
        self.nc = nc
        self.ops = []

    def add(self, eng, fn, reads=(), writes=(), dma=False):
        op = Op(eng, fn, reads, writes, dma)
        op.idx = len(self.ops)
        self.ops.append(op)
        return op

    def pe(self, fn, reads=(), writes=()):
        return self.add("pe", fn, reads, writes)

    def act(self, fn, reads=(), writes=()):
        return self.add("act", fn, reads, writes)

    def dve(self, fn, reads=(), writes=()):
        return self.add("dve", fn, reads, writes)

    def pool(self, fn, reads=(), writes=()):
        return self.add("pool", fn, reads, writes)

    def dma(self, out, in_, reads=(), writes=(), q="sp", **kw):
        return self.add(q, lambda e: e.dma_start(out=out, in_=in_, **kw), reads, writes, dma=True)

    def barrier(self):
        h = self.add("dve", None, (), ())
        h.barrier = "hub"
        scr = self.scr
        r1 = self.add("dve", lambda e: e.memset(scr, 0.0), (), ())
        r2 = self.add("dve", lambda e: e.memset(scr, 0.0), (), ())
        for e, r in (("pe", r1), ("act", r1), ("pool", r2), ("sp", r2)):
            b = self.add(e, None, (), ())
            b.barrier = ("wait", r)

    def analyze(self):
        last_w = {}
        readers = {}
        last_on_eng = {e: None for e in ENGS}
        pending_dma = []
        for op in self.ops:
            deps = []
            if op.barrier == "hub":
                for e in ENGS:
                    if last_on_eng[e] is not None and e != op.eng:
                        deps.append((last_on_eng[e], "bar"))
                for d in pending_dma:
                    deps.append((d, "bar"))
                pending_dma = []
                last_w.clear()
                readers.clear()
            elif op.barrier:
                deps.append((op.barrier[1], "bar"))
            else:
                op_nr = getattr(op.fn, "_nr", ())
                def conflicts(table, buf, key):
                    ent = table.get(buf)
                    if not ent:
                        return
                    if key is None:
                        for v in ent.values():
                            yield v
                    else:
                        if key in ent:
                            yield ent[key]
                        if None in ent:
                            yield ent[None]
                for (b, k) in op.reads:
                    for w in conflicts(last_w, b, k):
                        deps.append((w, "raw"))
                for (b, k) in op.writes:
                    for w in conflicts(last_w, b, k):
                        deps.append((w, "waw"))
                    for rl in conflicts(readers, b, k):
                        for r in rl.values():
                            deps.append((r, "war"))
                for (b, k) in op.writes:
                    ent = last_w.setdefault(b, {})
                    rent = readers.setdefault(b, {})
                    if k is None:
                        ent.clear()
                        rent.clear()
                    else:
                        rent.pop(k, None)
                    ent[k] = op
                for (b, k) in op.reads:
                    if (b, k) in op_nr:
                        continue
                    rd = readers.setdefault(b, {}).setdefault(k, {})
                    rd[("dma", op.idx) if op.dma else op.eng] = op
            fdeps = []
            seen = set()
            for p, kind in deps:
                if p is op:
                    continue
                if (not p.dma) and (not op.dma) and p.eng == op.eng:
                    if op.eng == "pe" or kind != "raw":
                        continue
                if p.idx in seen:
                    continue
                seen.add(p.idx)
                fdeps.append(p)
            op.deps = fdeps
            for p in fdeps:
                if not p.dma:
                    p.sig = True
                    if p.cons is None:
                        p.cons = set()
                    p.cons.add(op.eng)
            if op.dma:
                pending_dma.append(op)
            elif not op.barrier:
                last_on_eng[op.eng] = op

    def emit(self):
        nc = self.nc
        self.analyze()
        sigcnt = {}
        dcnt = {q: 0 for q in DMA_RING}
        for op in self.ops:
            if op.dma:
                n = DMA_RING[op.eng]
                c = dcnt[op.eng]
                op.dsem = (op.eng, c % n)
                op.dval = 16 * (c // n + 1)
                op.dprev = 16 * (c // n)
                dcnt[op.eng] += 1
            elif op.sig:
                op.sigidx = {}
                for ce in sorted(op.cons):
                    key = (op.eng, ce)
                    op.sigidx[ce] = sigcnt.get(key, 0)
                    sigcnt[key] = sigcnt.get(key, 0) + 1
        with ExitStack() as st:
            esems = {}
            for key, cntv in sigcnt.items():
                n = cntv // SIG_CHUNK + 1
                esems[key] = [st.enter_context(nc.semaphore(f"s_{key[0]}_{key[1]}_{i}")) for i in range(n)]
            dsems = {}
            for q, n in DMA_RING.items():
                for i in range(min(n, dcnt[q])):
                    dsems[(q, i)] = st.enter_context(nc.semaphore(f"s_dma_{q}_{i}"))
            block = st.enter_context(nc.Block())
            per_eng = {e: [op for op in self.ops if op.eng == e] for e in ENGS}

            def run(eng_name, eng):
                waited = {}

                def wait(key, sem, val):
                    if waited.get(key, 0) >= val:
                        return
                    waited[key] = val
                    eng.wait_ge(sem, val)

                for op in per_eng[eng_name]:
                    for p in op.deps:
                        if p.dma:
                            wait(("d", p.dsem), dsems[p.dsem], p.dval)
                        else:
                            c, v = divmod(p.sigidx[eng_name], SIG_CHUNK)
                            wait((p.eng, c), esems[(p.eng, eng_name)][c], v + 1)
                    if op.dma and op.dprev > 0:
                        wait(("d", op.dsem), dsems[op.dsem], op.dprev)
                    if op.barrier:
                        continue
                    ins = op.fn(eng)
                    if op.dma:
                        ins.then_inc(dsems[op.dsem], 16)
                    elif op.sig:
                        items = list(op.sigidx.items())
                        for n_, (ce, si) in enumerate(items):
                            if n_ >= 1:
                                if eng_name == "pe":
                                    assert getattr(op.fn, "_idem", False), f"non-idempotent PE op with consumers {items}"
                                    ins = op.fn(eng)
                                elif eng_name == "act":
                                    ins = eng.activation(out=self.scr_act, in_=self.scr_act, func=AF.Copy)
                                else:
                                    ins = eng.memset(self.scr_pool if eng_name == "pool" else self.scr, 0.0)
                            c, v = divmod(si, SIG_CHUNK)
                            ins.then_inc(esems[(op.eng, ce)][c], 1)

            @block.tensor
            def _(eng):
                run("pe", eng)

            @block.scalar
            def _(eng):
                run("act", eng)

            @block.vector
            def _(eng):
                run("dve", eng)

            @block.gpsimd
            def _(eng):
                run("pool", eng)

            @block.sync
            def _(eng):
                run("sp", eng)
        return sigcnt, dcnt


class SBAlloc:
    def __init__(self, nc, nwords):
        self.t = nc.alloc_sbuf_tensor("sbig", [128, nwords], F32)
        self.n = nwords
        self.off = 0

    def mark(self):
        return self.off

    def release(self, m):
        self.off = m

    def alloc(self, shape, dtype, name="t"):
        nelem = 1
        for s in shape[1:]:
            nelem *= s
        nbytes = nelem * (4 if dtype == F32 else 2)
        words = (nbytes + 3) // 4
        words = (words + 7) // 8 * 8
        assert self.off + words <= self.n, f"SBUF overflow allocating {name} {shape}: {self.off}+{words}>{self.n}"
        ap = self.t[0:shape[0], self.off:self.off + words]
        self.off += words
        if dtype != F32:
            ap = ap.bitcast(dtype)
        ap = ap[:, 0:nelem]
        if len(shape) == 3:
            ap = ap.rearrange("p (a b) -> p a b", a=shape[1], b=shape[2])
        elif len(shape) == 4:
            ap = ap.rearrange("p (a b c) -> p a b c", a=shape[1], b=shape[2], c=shape[3])
        return Tile(ap, name)


DEPTH = 2
S_LEN = 4096
DM = 1024
NT = 32
NG = 8
ALPHA = (2 * DEPTH) ** 0.25
INW = 4616
PG = [(0, 512), (512, 512), (1024, 448), (1472, 384), (1856, 384), (2240, 512), (2752, 512), (3264, 328),
      (3592, 512), (4104, 512)]
N_ALIBI = 10
SLOPES = [2.0 ** (-8.0 * (i + 1) / N_ALIBI) for i in range(N_ALIBI)]


PSTOP = 99
NGLIM = 8
EVMODE = 2


def build_nc(depth=DEPTH, debug=False, phases="PABCO"):
    nc = bass.Bass("TRN2", target_bir_lowering=False)
    S = Sched(nc)

    def din(name, shape, dt=F32):
        return nc.dram_tensor(name, list(shape), dt, kind="ExternalInput").ap()

    def dscr(name, shape, dt=BF16):
        if debug:
            return nc.dram_tensor(name, list(shape), dt, kind="ExternalOutput").ap()
        return nc.dram_tensor(name, list(shape), dt).ap()

    x_in = din("x", [S_LEN, DM])
    c_col = din("c_col", [128, 8])
    w_ada = din("w_ada", [depth, DM, 3 * DM])
    b_ada = din("b_ada", [depth, 1, 3 * DM])
    w_in = din("w_in", [depth, DM, INW])
    w_uq = din("w_uq", [depth, 768, 768])
    w_ukv = din("w_ukv", [depth, 256, 768])
    w_out = din("w_out", [depth, DM, DM])
    qg_col = din("qg_col", [depth, 128, 6])
    kvg_col = din("kvg_col", [depth, 128, 2])
    lng_b = din("lng_b", [depth, 128, DM])
    lnb_b = din("lnb_b", [depth, 128, DM])
    ident_f_d = din("ident_f", [128, 128])
    ident_b_d = din("ident_b", [128, 128], BF16)
    cs_d = din("cs_t", [128, NT, 32])
    sg_d = din("sg_t", [128, NT, 32])
    mtab_d = din("mtab", [128, 23 * 128], BF16)
    ctab_d = din("ctab", [128, 7 * 128], BF16)
    negm_d = din("negm", [128, 128])
    hp_d = din("halfpow", [128, 16])
    kpos_d = din("kpos", [N_ALIBI, 32, S_LEN], BF16)
    qpos_d = din("qpos", [N_ALIBI, 32, S_LEN], BF16)
    out_d = nc.dram_tensor("out", [S_LEN, DM], F32, kind="ExternalOutput").ap()

    QaT_d = dscr("QaT_d", [96, 6, S_LEN]); KaT_d = dscr("KaT_d", [96, 6, S_LEN])
    Va_d = dscr("Va_d", [S_LEN, 6 * 65])
    QbT_d = dscr("QbT_d", [64, 6, S_LEN]); KbT_d = dscr("KbT_d", [64, 6, S_LEN])
    Vb_d = dscr("Vb_d", [S_LEN, 6 * 65])
    QcT_d = dscr("QcT_d", [64, 4, S_LEN]); KcT_d = dscr("KcT_d", [64, 4, S_LEN])
    Vc_d = dscr("Vc_d", [S_LEN, 4 * 65])
    QiT_d = dscr("QiT_d", [128, 4, S_LEN]); KiT_d = dscr("KiT_d", [128, S_LEN])
    sgn_d = dscr("sgn_d", [S_LEN, 8], F32)
    gT_d = dscr("gT_d", [128, 8, S_LEN])
    yT_d = dscr("yT_d", [DM, S_LEN])
    xmid_d = dscr("xmid_d", [S_LEN, DM], F32)
    d_bufs = {n: Buf(n) for n in ["QaT", "KaT", "Va", "QbT", "KbT", "Vb", "QcT", "KcT", "Vc", "QiT", "KiT",
                                  "sgn", "gT", "yT", "xmid", "out"]}

    sb = SBAlloc(nc, 51500)
    ps = nc.alloc_psum_tensor("psum", [128, 4096], F32)
    S.scr = sb.alloc([1, 8], F32, "scr_dve").ap
    S.scr_act = sb.alloc([1, 8], F32, "scr_act").ap
    S.scr_pool = sb.alloc([1, 8], F32, "scr_pool").ap
    pbuf = [Buf(f"bank{i}") for i in range(8)]

    def bank(i):
        return ps[:, i * 512:(i + 1) * 512]

    def bank_bf(i):
        return ps[:, i * 512:(i + 1) * 512].bitcast(BF16)

    ident_f = sb.alloc([128, 128], F32, "ident_f")
    ident_b = sb.alloc([128, 128], BF16, "ident_b")
    cs_t = sb.alloc([128, NT, 32], F32, "cs")
    sg_t = sb.alloc([128, NT, 32], F32, "sg")
    mtab = sb.alloc([128, 23 * 128], BF16, "mtab")
    ctab = sb.alloc([128, 7 * 128], BF16, "ctab")
    negm = sb.alloc([128, 128], F32, "negm")
    halfpow = sb.alloc([128, 16], F32, "hp")
    ones_f = sb.alloc([128, 128], F32, "ones")
    ones_b = sb.alloc([128, 64], BF16, "ones_b")
    silu_c = sb.alloc([128, 8], F32, "siluc")
    c_sb = sb.alloc([128, 8], F32, "c")
    sc_col = sb.alloc([128, 8], F32, "sc_col")
    sh_col = sb.alloc([128, 8], F32, "sh_col")
    gate_b = sb.alloc([128, DM], F32, "gate_b")
    for t, d in [(ident_f, ident_f_d), (ident_b, ident_b_d), (cs_t, cs_d), (sg_t, sg_d), (mtab, mtab_d),
                 (ctab, ctab_d), (negm, negm_d), (halfpow, hp_d), (c_sb, c_col)]:
        S.dma(t.ap, d, writes=[t])
    S.dve(lambda e: e.memset(ones_f.ap, 1.0), writes=[ones_f])
    S.dve(lambda e: e.memset(S.scr, 0.0), writes=[Buf("scr")])
    S.dve(lambda e: e.memset(S.scr_act, 0.0), writes=[Buf("scra")])
    S.dve(lambda e: e.memset(S.scr_pool, 0.0), writes=[Buf("scrp")])
    S.dve(lambda e: e.memset(ones_b.ap, 1.0), writes=[ones_b])
    S.act(lambda e: e.activation(out=silu_c.ap, in_=c_sb.ap, func=AF.Silu), reads=[c_sb], writes=[silu_c])

    def mm(out, lhsT, rhs, start, stop, reads, writes, nr=()):
        o = S.pe(lambda e: e.matmul(out, lhsT=lhsT, rhs=rhs, start=start, stop=stop), reads, writes)
        o.fn._idem = bool(start and stop)
        if nr:
            o.fn._nr = tuple(_norm(x) for x in nr)
        return o

    def tr(out, in_, ident, reads, writes):
        o = S.pe(lambda e: e.transpose(out, in_, ident), reads, writes)
        o.fn._idem = True
        return o

    for l in range(depth):
        x_src = x_in if l == 0 else xmid_d
        x_src_buf = None if l == 0 else d_bufs["xmid"]
        x_dst = out_d if l == depth - 1 else xmid_d
        x_dst_buf = d_bufs["out"] if l == depth - 1 else d_bufs["xmid"]
        xr = [x_src_buf] if x_src_buf is not None else []

        m0 = sb.mark()
        wa = [sb.alloc([128, 3 * DM], F32, f"wa{i}") for i in range(2)]
        bada = sb.alloc([1, 3 * DM], F32, "bada")
        mod_row = sb.alloc([1, 3 * DM], F32, "mod_row")
        S.dma(bada.ap, b_ada[l], writes=[bada])
        for k in range(8):
            w_ = wa[k % 2]
            S.dma(w_.ap, w_ada[l, k * 128:(k + 1) * 128, :], writes=[w_])
            for cg in range(6):
                mm(bank(cg)[0:1, :], silu_c[:, k:k + 1], w_[:, cg * 512:(cg + 1) * 512], k == 0, k == 7,
                   [silu_c, w_], [pbuf[cg]])
        for cg in range(6):
            S.dve(lambda e, cg=cg: e.tensor_tensor(out=mod_row[0:1, cg * 512:(cg + 1) * 512], in0=bank(cg)[0:1, :],
                                                   in1=bada[0:1, cg * 512:(cg + 1) * 512], op=ALU.add),
                  reads=[pbuf[cg], bada], writes=[mod_row.k(cg)])
        for j in range(16):
            mm(bank(6)[:, 2 * j:2 * j + 2], mod_row[0:1, j * 128:(j + 1) * 128], ones_f[0:1, 0:2], True, True,
               [mod_row, ones_f], [pbuf[6]])
        colv = bank(6)[:, 0:32].rearrange("p (a b) -> p a b", b=2)
        S.dve(lambda e: e.tensor_copy(out=sh_col.ap, in_=colv[:, 0:8, 0]), reads=[pbuf[6]], writes=[sh_col])
        S.dve(lambda e: e.tensor_scalar(out=sc_col.ap, in0=colv[:, 8:16, 0], scalar1=1.0, scalar2=None, op0=ALU.add),
              reads=[pbuf[6]], writes=[sc_col])
        for cg in range(2):
            mm(bank(7), ones_f[0:1, 0:128], mod_row[0:1, 2048 + cg * 512:2048 + (cg + 1) * 512], True, True,
               [mod_row, ones_f], [pbuf[7]])
            S.dve(lambda e, cg=cg: e.tensor_scalar(out=gate_b[:, cg * 512:(cg + 1) * 512], in0=bank(7), scalar1=1.0,
                                                   scalar2=None, op0=ALU.add),
                  reads=[pbuf[7]], writes=[gate_b.k(cg)])
        S.barrier()
        sb.release(m0)

        if "P" in phases:
            m0 = sb.mark()
            w_in_sb = sb.alloc([128, 8, INW], BF16, "w_in")
            for k in range(8):
                for c3 in range(4):
                    c0, c1 = c3 * 1154, (c3 + 1) * 1154
                    S.dma(w_in_sb[:, k, c0:c1], w_in[l, k * 128:(k + 1) * 128, c0:c1], writes=[w_in_sb.k((k, c3))],
                          q="pool")
            wuq_sb = sb.alloc([128, 6, 768], BF16, "wuq")
            wukv_sb = sb.alloc([128, 2, 768], BF16, "wukv")
            qg_sb = sb.alloc([128, 6], F32, "qg")
            kvg_sb = sb.alloc([128, 2], F32, "kvg")
            S.dma(qg_sb.ap, qg_col[l], writes=[qg_sb])
            S.dma(kvg_sb.ap, kvg_col[l], writes=[kvg_sb])
            m1 = sb.mark()
            wst = sb.alloc([128, 8, 768], F32, "wst")
            S.dma(wst[:, 0:6, :], w_uq[l].rearrange("(k p) n -> p k n", p=128), writes=[wst.k("q")])
            S.dma(wst[:, 6:8, :], w_ukv[l].rearrange("(k p) n -> p k n", p=128), writes=[wst.k("kv")])
            for k in range(6):
                S.dve(lambda e, k=k: e.tensor_scalar(out=wuq_sb[:, k, :], in0=wst[:, k, :], scalar1=qg_sb[:, k:k + 1],
                                                     scalar2=None, op0=ALU.mult),
                      reads=[wst.k("q"), qg_sb], writes=[wuq_sb.k(k)])
            for k in range(2):
                S.dve(lambda e, k=k: e.tensor_scalar(out=wukv_sb[:, k, :], in0=wst[:, 6 + k, :],
                                                     scalar1=kvg_sb[:, k:k + 1], scalar2=None, op0=ALU.mult),
                      reads=[wst.k("kv"), kvg_sb], writes=[wukv_sb.k(k)])
            S.barrier()
            sb.release(m1)

            xt = [sb.alloc([128, DM], F32, f"xt{i}") for i in range(2)]
            hT = [sb.alloc([128, 8, 128], BF16, f"hT{i}") for i in range(2)]
            cq_bf = sb.alloc([128, 1024], BF16, "cq_bf")
            cT = sb.alloc([128, 8, 128], BF16, "cT")
            ssq = sb.alloc([128, 4], F32, "ssq")
            rv = sb.alloc([128, 2], F32, "rv")
            rr = sb.alloc([128, 2], F32, "rr")
            junk = sb.alloc([128, 512], F32, "junkP")
            krt = sb.alloc([128, 3, 32], F32, "krt")
            krope = sb.alloc([128, 32], BF16, "krope")
            qb_bf = sb.alloc([128, 384], BF16, "qb_bf")
            kb_bf = sb.alloc([128, 384], BF16, "kb_bf")
            qc_bf = sb.alloc([128, 256], BF16, "qc_bf")
            kc_bf = sb.alloc([128, 256], BF16, "kc_bf")
            qi_f = sb.alloc([128, 8, 64], F32, "qi_f")
            qi_bf = sb.alloc([128, 8, 64], BF16, "qi_bf")
            ki_dup = sb.alloc([128, 2, 64], BF16, "ki_dup")
            absw = sb.alloc([128, 8], F32, "absw")
            gate_bf = sb.alloc([128, DM], BF16, "gate_bf")
            qa_f = sb.alloc([128, 6, 128], F32, "qa_f")
            tmr = sb.alloc([128, 2, 6, 32], F32, "tmr")
            qA = sb.alloc([128, 6, 96], BF16, "qA")
            kA = sb.alloc([128, 6, 96], BF16, "kA")
            QaT_st = sb.alloc([96, 6, 512], BF16, "QaT_st"); KaT_st = sb.alloc([96, 6, 512], BF16, "KaT_st")
            QbT_st = sb.alloc([64, 6, 512], BF16, "QbT_st"); KbT_st = sb.alloc([64, 6, 512], BF16, "KbT_st")
            QcT_st = sb.alloc([64, 4, 512], BF16, "QcT_st"); KcT_st = sb.alloc([64, 4, 512], BF16, "KcT_st")
            QiT_st = sb.alloc([128, 4, 512], BF16, "QiT_st"); KiT_st = sb.alloc([128, 512], BF16, "KiT_st")
            gT_st = sb.alloc([128, 8, 512], BF16, "gT_st")
            Va_st = sb.alloc([128, 4, 6, 65], BF16, "Va_st"); Vb_st = sb.alloc([128, 4, 6, 65], BF16, "Vb_st")
            Vc_st = sb.alloc([128, 4, 4, 65], BF16, "Vc_st")
            sgn_st = sb.alloc([128, 4, 8], F32, "sgn_st")
            for vs in (Va_st, Vb_st, Vc_st):
                S.pool(lambda e, vs=vs: e.memset(vs.ap, 1.0), writes=[vs])

            def evac_copy(eng, out, in_, reads, writes, scale=None):
                if eng == "act":
                    if scale is None:
                        S.act(lambda e: e.activation(out=out, in_=in_, func=AF.Copy), reads, writes)
                    else:
                        S.act(lambda e: e.activation(out=out, in_=in_, func=AF.Copy, scale=scale), reads, writes)
                else:
                    if scale is None:
                        S.dve(lambda e: e.tensor_copy(out=out, in_=in_), reads, writes)
                    else:
                        S.dve(lambda e: e.tensor_scalar(out=out, in0=in_, scalar1=scale, scalar2=None, op0=ALU.mult),
                              reads, writes)

            trc = [0]

            def tgroup(srcs, src_reads, width, dst, dst_w, eng):
                b = (7, 0, 1)[trc[0] % 3]
                n = len(srcs)
                pv = bank_bf(b)
                for j, s_ap in enumerate(srcs):
                    tr(pv[0:width, j * 128:(j + 1) * 128], s_ap, ident_b.ap, list(src_reads) + [ident_b], [pbuf[b]])
                src = pv[0:width, 0:n * 128]
                if n > 1:
                    src = src.rearrange("p (a b) -> p a b", a=n, b=128)
                evac_copy(eng, dst, src, [pbuf[b]], dst_w)
                trc[0] += 1

            for g in range(min(NG, NGLIM) if PSTOP > 1 else 0):
                for i in range(4):
                    t = 4 * g + i
                    tsl = slice(i * 128, (i + 1) * 128)
                    xb = xt[t % 2]
                    hb = hT[t % 2]
                    S.dma(xb.ap, x_src[t * 128:(t + 1) * 128, :], reads=xr, writes=[xb])
                    for k in range(8):
                        bk = k // 4
                        o_ = tr(bank(bk)[:, (k % 4) * 128:(k % 4 + 1) * 128], xb[:, k * 128:(k + 1) * 128], ident_f.ap,
                                [xb, ident_f], [pbuf[bk]])
                        o_.fn._nr = ((xb.buf, None),)
                    for k in range(8):
                        bk = k // 4
                        src = bank(bk)[:, (k % 4) * 128:(k % 4 + 1) * 128]
                        if (k % 2 == 0 and EVMODE == 0) or EVMODE == 1 or (EVMODE == 3 and k < 4):
                            S.act(lambda e, k=k, src=src, hb=hb: e.activation(out=hb[:, k, :], in_=src, func=AF.Identity,
                                                                             scale=sc_col[:, k:k + 1],
                                                                             bias=sh_col[:, k:k + 1]),
                                  reads=[pbuf[bk], sc_col, sh_col, xb], writes=[hb.k(k)])
                        else:
                            S.dve(lambda e, k=k, src=src, hb=hb: e.tensor_scalar(out=hb[:, k, :], in0=src,
                                                                                scalar1=sc_col[:, k:k + 1],
                                                                                scalar2=sh_col[:, k:k + 1],
                                                                                op0=ALU.mult, op1=ALU.add),
                                  reads=[pbuf[bk], sc_col, sh_col, xb], writes=[hb.k(k)])
                    if PSTOP <= 2:
                        continue
                    for gi, (c0, cw) in enumerate(PG):
                        b = 2 + gi % 3
                        pb = bank(b)
                        for k in range(8):
                            mm(pb[:, 0:cw], hb[:, k, :], w_in_sb[:, k, c0:c0 + cw], k == 0, k == 7,
                               [hb, w_in_sb], [pbuf[b]], nr=[hb])
                        R = [pbuf[b]]
                        if gi == 0:
                            evac_copy("act", cq_bf[:, 0:512], pb, R, [cq_bf.k(0)])
                            S.act(lambda e: e.activation(out=junk.ap, in_=cq_bf[:, 0:512], func=AF.Square,
                                                         accum_out=ssq[:, 0:1]), [cq_bf.k(0)], [junk, ssq.k(0)])
                        elif gi == 1:
                            evac_copy("act", cq_bf[:, 512:1024], pb, R, [cq_bf.k(1)])
                            S.act(lambda e: e.activation(out=junk[:, 0:256], in_=cq_bf[:, 512:768], func=AF.Square,
                                                         accum_out=ssq[:, 1:2]), [cq_bf.k(1)], [junk, ssq.k(1)])
                            S.act(lambda e: e.activation(out=junk[:, 256:512], in_=cq_bf[:, 768:1024],
                                                         func=AF.Square, accum_out=ssq[:, 2:3]),
                                  [cq_bf.k(1)], [junk, ssq.k(2)])
                        elif gi == 2:
                            evac_copy("act", krt[:, 2, :], pb[:, 0:32], R, [krt.k(2)])
                            evac_copy("act", krt[:, 1, :], pb[:, 32:64], R, [krt.k(1)])
                            S.dve(lambda e, t=t: e.tensor_tensor(out=krt[:, 0, :], in0=krt[:, 2, :],
                                                                 in1=cs_t[:, t, :], op=ALU.mult),
                                  [krt.k(2), cs_t], [krt.k(0)])
                            S.dve(lambda e, t=t: e.tensor_tensor(out=krt[:, 1, :], in0=krt[:, 1, :],
                                                                 in1=sg_t[:, t, :], op=ALU.mult),
                                  [krt.k(1), sg_t], [krt.k(1)])
                            S.dve(lambda e: e.tensor_tensor(out=krope.ap, in0=krt[:, 0, :], in1=krt[:, 1, :], op=ALU.add),
                                  [krt], [krope])
                            evac_copy("act", qb_bf.ap, pb[:, 64:448], R, [qb_bf], scale=0.125)
                        elif gi == 3:
                            evac_copy("dve", kb_bf.ap, pb[:, 0:384], R, [kb_bf])
                        elif gi == 4:
                            evac_copy("dve", Vb_st[:, i, :, 0:64],
                                      pb[:, 0:384].rearrange("p (a b) -> p a b", a=6, b=64), R, [Vb_st.k(i)])
                        elif gi == 5:
                            evac_copy("dve", qc_bf.ap, pb[:, 0:256], R, [qc_bf], scale=0.125)
                            evac_copy("dve", kc_bf.ap, pb[:, 256:512], R, [kc_bf])
                        elif gi == 6:
                            evac_copy("act", Vc_st[:, i, :, 0:64],
                                      pb[:, 0:256].rearrange("p (a b) -> p a b", a=4, b=64), R, [Vc_st.k(i)])
                            evac_copy("act", qi_f[:, 0:4, :], pb[:, 256:512].rearrange("p (a b) -> p a b", a=4, b=64),
                                      R, [qi_f.k(0)])
                        elif gi == 7:
                            evac_copy("act", qi_f[:, 4:8, :], pb[:, 0:256].rearrange("p (a b) -> p a b", a=4, b=64),
                                      R, [qi_f.k(1)])
                            evac_copy("act", ki_dup[:, 0, :], pb[:, 256:320], R, [ki_dup.k(0)])
                            evac_copy("act", ki_dup[:, 1, :], pb[:, 256:320], R, [ki_dup.k(1)])
                            S.act(lambda e, pb=pb: e.activation(out=absw.ap, in_=pb[:, 320:328], func=AF.Abs,
                                                                scale=1.0 / (8.0 * math.sqrt(8.0))), R, [absw])
                            S.act(lambda e, pb=pb, i=i: e.activation(out=sgn_st[:, i, :], in_=pb[:, 320:328],
                                                                     func=AF.Sign), R, [sgn_st.k(i)])
                            S.dve(lambda e: e.tensor_tensor(out=qi_bf.ap, in0=qi_f.ap,
                                                            in1=absw.ap.unsqueeze(2).to_broadcast([128, 8, 64]),
                                                            op=ALU.mult), [qi_f, absw], [qi_bf])
                        else:
                            h0 = (gi - 8) * 512
                            S.act(lambda e, pb=pb, h0=h0: e.activation(out=gate_bf[:, h0:h0 + 512], in_=pb,
                                                                       func=AF.Silu), R + [hb], [gate_bf.k(gi)])
                    if PSTOP <= 3:
                        continue
                    S.dve(lambda e: e.tensor_tensor(out=rv[:, 0:1], in0=ssq[:, 0:1], in1=ssq[:, 1:2], op=ALU.add),
                          [ssq], [rv.k(0)])
                    S.dve(lambda e: e.tensor_scalar(out=rv[:, 0:1], in0=rv[:, 0:1], scalar1=1.0 / 8.0, scalar2=96e-6,
                                                    op0=ALU.mult, op1=ALU.add), [rv.k(0)], [rv.k(0)])
                    S.dve(lambda e: e.tensor_scalar(out=rv[:, 1:2], in0=ssq[:, 2:3], scalar1=1.0 / 256.0, scalar2=1e-6,
                                                    op0=ALU.mult, op1=ALU.add), [ssq], [rv.k(1)])
                    S.act(lambda e: e.activation(out=rr.ap, in_=rv.ap, func=AF.Sqrt), [rv], [rr])
                    S.dve(lambda e: e.reciprocal(out=rr.ap, in_=rr.ap), [rr], [rr])
                    tgroup([cq_bf[:, k * 128:(k + 1) * 128] for k in range(8)], [cq_bf], 128, cT.ap, [cT], "act")
                    for cg in range(2):
                        b = 5 + cg
                        for k in range(6):
                            mm(bank(b)[:, 0:384], cT[:, k, :], wuq_sb[:, k, cg * 384:(cg + 1) * 384], k == 0, k == 5,
                               [cT, wuq_sb], [pbuf[b]])
                        S.act(lambda e, b=b, cg=cg: e.activation(
                            out=qa_f[:, cg * 3:(cg + 1) * 3, :],
                            in_=bank(b)[:, 0:384].rearrange("p (a b) -> p a b", a=3, b=128),
                            func=AF.Identity, scale=rr[:, 0:1]), [pbuf[b], rr], [qa_f.k(cg)])
                    S.pool(lambda e: e.tensor_copy(out=qA[:, :, 0:64], in_=qa_f[:, :, 0:64]), [qa_f], [qA.k("n")])
                    S.pool(lambda e, t=t: e.tensor_tensor(out=tmr[:, 0, :, :], in0=qa_f[:, :, 64:96],
                                                          in1=cs_t[:, t:t + 1, :].to_broadcast([128, 6, 32]),
                                                          op=ALU.mult), [qa_f, cs_t], [tmr.k(0)])
                    S.pool(lambda e, t=t: e.tensor_tensor(out=tmr[:, 1, :, :], in0=qa_f[:, :, 96:128],
                                                          in1=sg_t[:, t:t + 1, :].to_broadcast([128, 6, 32]),
                                                          op=ALU.mult), [qa_f, sg_t], [tmr.k(1)])
                    S.pool(lambda e: e.tensor_tensor(out=qA[:, :, 64:96], in0=tmr[:, 0, :, :], in1=tmr[:, 1, :, :],
                                                     op=ALU.add), [tmr], [qA.k("r")])
                    for cg in range(2):
                        b = 5 + cg
                        for k in range(2):
                            mm(bank(b)[:, 0:384], cT[:, 6 + k, :], wukv_sb[:, k, cg * 384:(cg + 1) * 384], k == 0,
                               k == 1, [cT, wukv_sb], [pbuf[b]])
                        dst = kA[:, :, 0:64] if cg == 0 else Va_st[:, i, :, 0:64]
                        dw = [kA.k("n")] if cg == 0 else [Va_st.k(i)]
                        S.act(lambda e, b=b, dst=dst: e.activation(
                            out=dst, in_=bank(b)[:, 0:384].rearrange("p (a b) -> p a b", a=6, b=64),
                            func=AF.Identity, scale=rr[:, 1:2]), [pbuf[b], rr], dw)
                    S.pool(lambda e: e.tensor_copy(out=kA[:, :, 64:96],
                                                   in_=krope.ap.unsqueeze(1).to_broadcast([128, 6, 32])),
                           [krope], [kA.k("r")])
                    if PSTOP <= 4:
                        continue
                    tgroup([qA[:, h, :] for h in range(6)], [qA], 96, QaT_st[:, :, tsl], [QaT_st.k(i)], "act")
                    tgroup([kA[:, h, :] for h in range(6)], [kA], 96, KaT_st[:, :, tsl], [KaT_st.k(i)], "dve")
                    tgroup([qb_bf[:, h * 64:(h + 1) * 64] for h in range(6)], [qb_bf], 64, QbT_st[:, :, tsl],
                           [QbT_st.k(i)], "act")
                    tgroup([kb_bf[:, h * 64:(h + 1) * 64] for h in range(6)], [kb_bf], 64, KbT_st[:, :, tsl],
                           [KbT_st.k(i)], "dve")
                    tgroup([qc_bf[:, h * 64:(h + 1) * 64] for h in range(4)], [qc_bf], 64, QcT_st[:, :, tsl],
                           [QcT_st.k(i)], "act")
                    tgroup([kc_bf[:, h * 64:(h + 1) * 64] for h in range(4)], [kc_bf], 64, KcT_st[:, :, tsl],
                           [KcT_st.k(i)], "dve")
                    tgroup([qi_bf[:, 2 * j:2 * j + 2, :].rearrange("p a b -> p (a b)") for j in range(4)], [qi_bf], 128,
                           QiT_st[:, :, tsl], [QiT_st.k(i)], "act")
                    tgroup([ki_dup.ap.rearrange("p a b -> p (a b)")], [ki_dup], 128, KiT_st[:, tsl], [KiT_st.k(i)],
                           "act")
                    tgroup([gate_bf[:, c * 128:(c + 1) * 128] for c in range(8)], [gate_bf], 128, gT_st[:, :, tsl],
                           [gT_st.k(i)], "dve")
                if PSTOP <= 5:
                    continue
                gs = slice(g * 512, (g + 1) * 512)
                rows = slice(g * 512, (g + 1) * 512)
                for st_, dd, nm in [(QaT_st, QaT_d, "QaT"), (KaT_st, KaT_d, "KaT"), (QbT_st, QbT_d, "QbT"),
                                    (KbT_st, KbT_d, "KbT"), (QcT_st, QcT_d, "QcT"), (KcT_st, KcT_d, "KcT"),
                                    (QiT_st, QiT_d, "QiT"), (gT_st, gT_d, "gT")]:
                    S.dma(dd[:, :, gs], st_.ap, reads=[st_], writes=[d_bufs[nm].__getitem__(g)])
                S.dma(KiT_d[:, gs], KiT_st.ap, reads=[KiT_st], writes=[d_bufs["KiT"][g]])
                for st_, dd, nm, hh in [(Va_st, Va_d, "Va", 6), (Vb_st, Vb_d, "Vb", 6), (Vc_st, Vc_d, "Vc", 4)]:
                    S.dma(dd[rows, :].rearrange("(t p) c -> p t c", p=128),
                          st_.ap.rearrange("p t h c -> p t (h c)"), reads=[st_], writes=[d_bufs[nm][g]])
                S.dma(sgn_d[rows, :].rearrange("(t p) c -> p t c", p=128), sgn_st.ap, reads=[sgn_st],
                      writes=[d_bufs["sgn"][g]])
            S.barrier()
            sb.release(m0)

        def attend_dense(KT_d, QT_d, V_d, nmK, nmQ, nmV, H, Dk, aug_base, kind, yoff):
            m0 = sb.mark()
            DK = Dk + (32 if aug_base is not None else 0)
            Vall = sb.alloc([128, NT, H * 65], BF16, "Vall")
            for t8 in range(8):
                S.dma(Vall[:, t8 * 4:(t8 + 1) * 4, :],
                      V_d[t8 * 512:(t8 + 1) * 512, :].rearrange("(t p) c -> p t c", p=128),
                      reads=[d_bufs[nmV]], writes=[Vall.k(t8)])
            KT = [sb.alloc([DK, S_LEN], BF16, f"KT{i}") for i in range(2)]
            QT = [sb.alloc([DK, S_LEN], BF16, f"QT{i}") for i in range(2)]
            PT = [sb.alloc([128, 512], BF16, f"PT{i}") for i in range(3)]
            gTt = [sb.alloc([64, 512], BF16, f"gTt{i}") for i in range(2)]
            num_sb = sb.alloc([64, 512], F32, "num_sb")
            rden = sb.alloc([65, 512], F32, "rden")
            y1 = sb.alloc([64, 512], F32, "y1")
            yst = [sb.alloc([64, 512], BF16, f"yst{i}") for i in range(2)]
            rcnt = 0
            gcnt = 0
            for h in range(H):
                kt_, qt_ = KT[h % 2], QT[h % 2]
                S.dma(kt_[0:Dk, :], KT_d[:, h, :], reads=[d_bufs[nmK]], writes=[kt_.k("d")])
                S.dma(qt_[0:Dk, :], QT_d[:, h, :], reads=[d_bufs[nmQ]], writes=[qt_.k("d")])
                if aug_base is not None:
                    S.dma(kt_[Dk:DK, :], kpos_d[aug_base + h], writes=[kt_.k("a")])
                    S.dma(qt_[Dk:DK, :], qpos_d[aug_base + h], writes=[qt_.k("a")])
                for g in range(NG):
                    if kind == "causal":
                        kts = list(range(0, 4 * g + 4))
                    else:
                        kts = list(range(max(0, 4 * g - 16), 4 * g + 4))
                    acc_b = 3 + gcnt % 2
                    den_b = 5 + gcnt % 2
                    gt = gTt[gcnt % 2]
                    ys = yst[gcnt % 2]
                    row0 = yoff + h * 64
                    ch, r0 = row0 // 128, row0 % 128
                    S.dma(gt.ap, gT_d[r0:r0 + 64, ch, g * 512:(g + 1) * 512], reads=[d_bufs["gT"]], writes=[gt])
                    qsl = slice(g * 512, (g + 1) * 512)

                    def c0_of(kt):
                        return max(0, kt - 4 * g) * 128

                    def score(j, r):
                        kt = kts[j]
                        c0 = c0_of(kt)
                        mm(bank(r % 3)[:, c0:512], kt_[0:DK, kt * 128:(kt + 1) * 128],
                           qt_[0:DK, g * 512 + c0:(g + 1) * 512], True, True, [kt_, qt_], [pbuf[r % 3]])

                    LA = 2
                    for j in range(min(LA, len(kts))):
                        score(j, rcnt + j)
                    for j, kt in enumerate(kts):
                        r = rcnt + j
                        p_ = PT[r % 3]
                        c0 = c0_of(kt)
                        S.act(lambda e, r=r, p_=p_, c0=c0: e.activation(out=p_[:, c0:512], in_=bank(r % 3)[:, c0:512],
                                                                        func=AF.Exp), [pbuf[r % 3]], [p_])
                        j0 = 4 * g - kt
                        if kind == "dilated":
                            msl = mtab[:, (j0 + 3) * 128 + c0:(j0 + 7) * 128]
                            S.dve(lambda e, p_=p_, msl=msl, c0=c0: e.tensor_tensor(out=p_[:, c0:512], in0=p_[:, c0:512],
                                                                                  in1=msl, op=ALU.mult),
                                  [p_, mtab], [p_])
                        elif j0 < 1:
                            msl = ctab[:, (j0 + 3) * 128 + c0:(j0 + 7) * 128]
                            S.dve(lambda e, p_=p_, msl=msl, c0=c0: e.tensor_tensor(out=p_[:, c0:512], in0=p_[:, c0:512],
                                                                                  in1=msl, op=ALU.mult),
                                  [p_, ctab], [p_])
                        if j + LA < len(kts):
                            score(j + LA, r + LA)
                        last = (j == len(kts) - 1)
                        mm(bank(acc_b)[0:64, c0:512], Vall[:, kt, h * 65:h * 65 + 64], p_[:, c0:512], j == 0, last,
                           [Vall, p_], [pbuf[acc_b]], nr=([p_] if last else ()))
                        mm(bank(den_b)[0:64, c0:512], ones_b.ap, p_[:, c0:512], j == 0, last,
                           [ones_b, p_], [pbuf[den_b]], nr=([p_] if last else ()))
                        p_last = p_
                    rcnt += len(kts)
                    S.dve(lambda e, den_b=den_b: e.reciprocal(out=num_sb.ap, in_=bank(den_b)[0:64, :]),
                          [pbuf[den_b], p_last], [num_sb])
                    S.dve(lambda e, acc_b=acc_b: e.tensor_tensor(out=y1.ap, in0=bank(acc_b)[0:64, :], in1=num_sb.ap,
                                                                 op=ALU.mult), [pbuf[acc_b], num_sb], [y1])
                    S.dve(lambda e, ys=ys, gt=gt: e.tensor_tensor(out=ys.ap, in0=y1.ap, in1=gt.ap, op=ALU.mult),
                          [y1, gt], [ys])
                    S.dma(yT_d[row0:row0 + 64, qsl], ys.ap, reads=[ys], writes=[d_bufs["yT"][(row0, g)]])
                    gcnt += 1
            S.barrier()
            sb.release(m0)

        if "A" in phases:
            attend_dense(KaT_d, QaT_d, Va_d, "KaT", "QaT", "Va", 6, 96, None, "causal", 0)
        if "B" in phases:
            attend_dense(KbT_d, QbT_d, Vb_d, "KbT", "QbT", "Vb", 6, 64, 0, "dilated", 384)

        if "C" in phases:
            m0 = sb.mark()
            KTc = sb.alloc([96, 4, S_LEN], BF16, "KTc")
            S.dma(KTc[0:64, :, :], KcT_d, reads=[d_bufs["KcT"]], writes=[KTc.k("d")])
            for h in range(4):
                S.dma(KTc[64:96, h, :], kpos_d[6 + h], writes=[KTc.k(("a", h))])
            Vc = sb.alloc([128, NT, 260], BF16, "Vc")
            for t8 in range(8):
                S.dma(Vc[:, t8 * 4:(t8 + 1) * 4, :],
                      Vc_d[t8 * 512:(t8 + 1) * 512, :].rearrange("(t p) c -> p t c", p=128),
                      reads=[d_bufs["Vc"]], writes=[Vc.k(t8)])
            KiT = sb.alloc([128, S_LEN], BF16, "KiT")
            S.dma(KiT.ap, KiT_d, reads=[d_bufs["KiT"]], writes=[KiT])
            QTc = [sb.alloc([96, 4, 128], BF16, f"QTc{i}") for i in range(2)]
            QiT = [sb.alloc([128, 4, 128], BF16, f"QiT{i}") for i in range(2)]
            sgn = [sb.alloc([128, 8], F32, f"sgn{i}") for i in range(2)]
            gTc = [sb.alloc([64, 4, 128], BF16, f"gTc{i}") for i in range(2)]
            score = sb.alloc([128, S_LEN], F32, "score")
            junkb = sb.alloc([128, S_LEN], BF16, "junkb")
            maskb = sb.alloc([128, S_LEN], BF16, "maskb")
            mT = sb.alloc([128, S_LEN], BF16, "mT")
            rl = [sb.alloc([128, 512], BF16, f"rl{i}") for i in range(3)]
            PTc = [sb.alloc([128, 512], BF16, f"PTc{i}") for i in range(2)]
            m8 = sb.alloc([128, 8], F32, "m8")
            lo = sb.alloc([128, 1], F32, "lo")
            w0 = sb.alloc([128, 1], F32, "w0")
            whalf = sb.alloc([128, 16], F32, "whalf")
            mid = sb.alloc([128, 1], F32, "mid")
            cnt = sb.alloc([128, 1], F32, "cnt")
            ssp = sb.alloc([128, 1], F32, "ssp")
            junka = sb.alloc([128, S_LEN // 2 + 128], BF16, "junka")
            stp = sb.alloc([128, 1], F32, "stp")
            numc = sb.alloc([64, 128], F32, "numc")
            rdenc = sb.alloc([65, 128], F32, "rdenc")
            y1c = sb.alloc([64, 128], F32, "y1c")
            ystc = [sb.alloc([64, 4, 128], BF16, f"ystc{i}") for i in range(2)]
            NIT = 16
            ir = 0
            sr = 0
            for qt in range(NT):
                n = qt + 1
                N = 128 * n
                qsl = slice(qt * 128, (qt + 1) * 128)
                Q_ = QTc[qt % 2]; Qi_ = QiT[qt % 2]; sg_ = sgn[qt % 2]; gt = gTc[qt % 2]; ys = ystc[qt % 2]
                S.dma(Q_[0:64, :, :], QcT_d[:, :, qsl], reads=[d_bufs["QcT"]], writes=[Q_.k("d")])
                for h in range(4):
                    S.dma(Q_[64:96, h, :], qpos_d[6 + h][:, qsl], writes=[Q_.k(("a", h))])
                S.dma(Qi_.ap, QiT_d[:, :, qsl], reads=[d_bufs["QiT"]], writes=[Qi_])
                S.dma(sg_.ap, sgn_d[qsl, :], reads=[d_bufs["sgn"]], writes=[sg_])
                for c2 in range(2):
                    S.dma(gt[:, 2 * c2:2 * c2 + 2, :], gT_d[:, 6 + c2, qsl].rearrange("(a p) t -> p a t", p=64),
                          reads=[d_bufs["gT"]], writes=[gt.k(c2)])
                nch = (N + 511) // 512
                for c in range(nch):
                    w = min(512, N - 512 * c)
                    csl = slice(c * 512, c * 512 + w)
                    for h in range(8):
                        b = ir % 3
                        pr = (h % 2) * 64
                        mm(bank(b)[:, 0:w], Qi_[pr:pr + 64, h // 2, :], KiT[pr:pr + 64, csl], True, True,
                           [Qi_, KiT], [pbuf[b]])
                        r_ = rl[ir % 3]
                        S.act(lambda e, b=b, w=w, r_=r_: e.activation(out=r_[:, 0:w], in_=bank(b)[:, 0:w], func=AF.Relu),
                              [pbuf[b]], [r_])
                        if h == 0:
                            S.dve(lambda e, r_=r_, w=w, csl=csl, sg_=sg_: e.tensor_scalar(
                                out=score[:, csl], in0=r_[:, 0:w], scalar1=sg_[:, 0:1], scalar2=None, op0=ALU.mult),
                                [r_, sg_], [score.k(c)])
                        else:
                            S.dve(lambda e, r_=r_, w=w, csl=csl, sg_=sg_, h=h: e.scalar_tensor_tensor(
                                out=score[:, csl], in0=r_[:, 0:w], scalar=sg_[:, h:h + 1], in1=score[:, csl],
                                op0=ALU.mult, op1=ALU.add), [r_, sg_, score.k(c)], [score.k(c)])
                        ir += 1
                thr = lo
                if qt >= 2:
                    S.dve(lambda e, N=N: e.tensor_reduce(out=lo.ap, in_=score[:, 0:N], axis=AX.X, op=ALU.min),
                          [score], [lo])
                S.dve(lambda e, N=N: e.tensor_tensor(out=score[:, N - 128:N], in0=score[:, N - 128:N], in1=negm.ap,
                                                     op=ALU.add), [score, negm], [score])
                if qt >= 2:
                    S.dve(lambda e, N=N: e.max(out=m8.ap, in_=score[:, 0:N]), [score], [m8])
                    S.dve(lambda e: e.tensor_tensor(out=w0.ap, in0=m8[:, 0:1], in1=lo.ap, op=ALU.subtract),
                          [m8, lo], [w0])
                    S.dve(lambda e: e.tensor_scalar(out=whalf.ap, in0=halfpow.ap, scalar1=w0[:, 0:1], scalar2=None,
                                                    op0=ALU.mult), [halfpow, w0], [whalf])
                    S.dve(lambda e: e.tensor_tensor(out=mid.ap, in0=lo.ap, in1=whalf[:, 0:1], op=ALU.add),
                          [lo, whalf], [mid])
                    N1 = ((n + 1) // 2) * 128
                    N2 = N - N1
                    thrN = 256.0 - N2 / 2.0
                    for it in range(NIT):
                        S.act(lambda e, N1=N1, N=N, N2=N2: e.activation(out=junka[:, 0:N2], in_=score[:, N1:N],
                                                                       func=AF.Sign, scale=-1.0, bias=mid[:, 0:1],
                                                                       accum_out=ssp[:, 0:1]),
                              [score, mid], [junka, ssp])
                        S.dve(lambda e, N1=N1: e.tensor_scalar(out=junkb[:, 0:N1], in0=score[:, 0:N1],
                                                               scalar1=mid[:, 0:1], scalar2=None, op0=ALU.is_ge,
                                                               op1=ALU.add, accum_out=cnt[:, 0:1]),
                              [score, mid], [junkb, cnt])
                        S.dve(lambda e: e.scalar_tensor_tensor(out=stp.ap, in0=ssp.ap, scalar=-0.5, in1=cnt.ap,
                                                               op0=ALU.mult, op1=ALU.add), [ssp, cnt], [stp])
                        lastit = (it == NIT - 1)
                        S.dve(lambda e, lastit=lastit, thrN=thrN: e.tensor_scalar(
                            out=stp.ap, in0=stp.ap, scalar1=float(thrN), scalar2=(-1.0 if lastit else -0.5),
                            op0=ALU.is_ge, op1=ALU.add), [stp], [stp])
                        dst = lo if lastit else mid
                        S.dve(lambda e, it=it, dst=dst: e.scalar_tensor_tensor(
                            out=dst.ap, in0=stp.ap, scalar=whalf[:, it:it + 1], in1=mid.ap, op0=ALU.mult,
                            op1=ALU.add), [stp, whalf, mid], [dst])
                else:
                    S.dve(lambda e: e.memset(lo.ap, -1e29), [], [lo])
                S.dve(lambda e, N=N: e.tensor_scalar(out=maskb[:, 0:N], in0=score[:, 0:N], scalar1=thr[:, 0:1],
                                                     scalar2=None, op0=ALU.is_ge), [score, lo], [maskb])
                nkb = (n + 3) // 4
                for kb in range(nkb):
                    nb = min(4, n - 4 * kb)
                    pv = bank_bf(7)
                    for j in range(nb):
                        kt = 4 * kb + j
                        tr(pv[:, j * 128:(j + 1) * 128], maskb[:, kt * 128:(kt + 1) * 128], ident_b.ap,
                           [maskb, ident_b], [pbuf[7]])
                    S.act(lambda e, pv=pv, kb=kb, nb=nb: e.activation(out=mT[:, kb * 512:kb * 512 + nb * 128],
                                                                      in_=pv[:, 0:nb * 128], func=AF.Copy),
                          [pbuf[7]], [mT.k(kb)])
                for h in range(4):
                    for kb in range(nkb):
                        nb = min(4, n - 4 * kb)
                        b = 3 + sr % 2
                        p_ = PTc[sr % 2]
                        for j in range(nb):
                            kt = 4 * kb + j
                            mm(bank(b)[:, j * 128:(j + 1) * 128], KTc[0:96, h, kt * 128:(kt + 1) * 128], Q_[0:96, h, :],
                               True, True, [KTc, Q_], [pbuf[b]])
                        S.act(lambda e, b=b, nb=nb, p_=p_: e.activation(out=p_[:, 0:nb * 128], in_=bank(b)[:, 0:nb * 128],
                                                                        func=AF.Exp), [pbuf[b]], [p_])
                        S.dve(lambda e, p_=p_, nb=nb, kb=kb: e.tensor_tensor(
                            out=p_[:, 0:nb * 128], in0=p_[:, 0:nb * 128], in1=mT[:, kb * 512:kb * 512 + nb * 128],
                            op=ALU.mult), [p_, mT.k(kb)], [p_])
                        for j in range(nb):
                            kt = 4 * kb + j
                            mm(bank(5)[0:64, 0:128], Vc[:, kt, h * 65:h * 65 + 64], p_[:, j * 128:(j + 1) * 128],
                               kt == 0, kt == n - 1, [Vc, p_], [pbuf[5]], nr=([p_] if kb == nkb - 1 else ()))
                            mm(bank(6)[0:64, 0:128], ones_b.ap, p_[:, j * 128:(j + 1) * 128],
                               kt == 0, kt == n - 1, [ones_b, p_], [pbuf[6]], nr=([p_] if kb == nkb - 1 else ()))
                            p_last = p_
                        sr += 1
                    S.dve(lambda e: e.reciprocal(out=numc.ap, in_=bank(6)[0:64, 0:128]), [pbuf[6], p_last], [numc])
                    S.dve(lambda e: e.tensor_tensor(out=y1c.ap, in0=bank(5)[0:64, 0:128], in1=numc.ap, op=ALU.mult),
                          [pbuf[5], numc], [y1c])
                    S.dve(lambda e, ys=ys, gt=gt, h=h: e.tensor_tensor(out=ys[:, h, :], in0=y1c.ap, in1=gt[:, h, :],
                                                                       op=ALU.mult), [y1c, gt], [ys.k(h)])
                S.dma(yT_d[768:1024, qsl].rearrange("(h p) t -> p h t", p=64), ys.ap, reads=[ys],
                      writes=[d_bufs["yT"][("c", qt)]])
            S.barrier()
            sb.release(m0)

        if "O" in phases:
            m0 = sb.mark()
            w_out_sb = sb.alloc([128, 8, DM], BF16, "w_out")
            for k in range(8):
                S.dma(w_out_sb[:, k, :], w_out[l, k * 128:(k + 1) * 128, :], writes=[w_out_sb.k(k)], q="pool")
            lng = sb.alloc([128, DM], F32, "lng")
            lnb = sb.alloc([128, DM], F32, "lnb")
            S.dma(lng.ap, lng_b[l], writes=[lng])
            S.dma(lnb.ap, lnb_b[l], writes=[lnb])
            yTs = [sb.alloc([128, 8, 512], BF16, f"yTs{i}") for i in range(2)]
            xo = [sb.alloc([128, DM], F32, f"xo{i}") for i in range(2)]
            zt = [sb.alloc([128, DM], F32, f"zt{i}") for i in range(2)]
            ot = [sb.alloc([128, DM], F32, f"ot{i}") for i in range(2)]
            stats = sb.alloc([128, 2, 6], F32, "stats")
            mv = sb.alloc([128, 2], F32, "mv")
            rs = sb.alloc([128, 1], F32, "rs")
            for g in range(NG):
                yt_ = yTs[g % 2]
                S.dma(yt_.ap, yT_d[:, g * 512:(g + 1) * 512].rearrange("(c p) t -> p c t", p=128),
                      reads=[d_bufs["yT"]], writes=[yt_])
                for i in range(4):
                    t = 4 * g + i
                    x_ = xo[t % 2]; z_ = zt[t % 2]; o_ = ot[t % 2]
                    S.dma(x_.ap, x_src[t * 128:(t + 1) * 128, :], reads=xr, writes=[x_])
                    for cg in range(2):
                        b = (2 * t + cg) % 4
                        for k in range(8):
                            mm(bank(b), yt_[:, k, i * 128:(i + 1) * 128], w_out_sb[:, k, cg * 512:(cg + 1) * 512],
                               k == 0, k == 7, [yt_, w_out_sb], [pbuf[b]], nr=[yt_])
                        csl = slice(cg * 512, (cg + 1) * 512)
                        S.dve(lambda e, b=b, z_=z_, csl=csl: e.tensor_tensor(out=z_[:, csl], in0=bank(b),
                                                                            in1=gate_b[:, csl], op=ALU.mult),
                              [pbuf[b], gate_b, yt_], [z_.k(cg)])
                        S.dve(lambda e, z_=z_, x_=x_, csl=csl: e.scalar_tensor_tensor(
                            out=z_[:, csl], in0=x_[:, csl], scalar=float(ALPHA), in1=z_[:, csl], op0=ALU.mult,
                            op1=ALU.add), [x_, z_.k(cg)], [z_.k(cg)])
                        S.dve(lambda e, z_=z_, csl=csl, cg=cg: e.bn_stats(out=stats[:, cg, :], in_=z_[:, csl]),
                              [z_.k(cg)], [stats.k(cg)])
                    S.dve(lambda e: e.bn_aggr(out=mv.ap, in_=stats.ap.rearrange("p a b -> p (a b)")), [stats], [mv])
                    S.dve(lambda e: e.tensor_scalar(out=rs.ap, in0=mv[:, 1:2], scalar1=1e-5, scalar2=None, op0=ALU.add),
                          [mv], [rs])
                    S.act(lambda e: e.activation(out=rs.ap, in_=rs.ap, func=AF.Sqrt), [rs], [rs])
                    S.dve(lambda e: e.reciprocal(out=rs.ap, in_=rs.ap), [rs], [rs])
                    S.dve(lambda e, z_=z_: e.tensor_scalar(out=z_.ap, in0=z_.ap, scalar1=mv[:, 0:1], scalar2=rs[:, 0:1],
                                                           op0=ALU.subtract, op1=ALU.mult), [z_, mv, rs], [z_])
                    S.pool(lambda e, z_=z_, o_=o_: e.tensor_tensor(out=o_.ap, in0=z_.ap, in1=lng.ap, op=ALU.mult),
                           [z_, lng], [o_])
                    S.pool(lambda e, o_=o_: e.tensor_tensor(out=o_.ap, in0=o_.ap, in1=lnb.ap, op=ALU.add),
                           [o_, lnb], [o_])
                    S.dma(x_dst[t * 128:(t + 1) * 128, :], o_.ap, reads=[o_], writes=[x_dst_buf[t]])
            S.barrier()
            sb.release(m0)

    S.barrier()
    info = S.emit()
    return nc, info


def _consts():
    bf = ml_dtypes.bfloat16
    cst = {}
    cst["ident_f"] = np.eye(128, dtype=np.float32)
    cst["ident_b"] = np.eye(128, dtype=np.float32).astype(bf)
    pos = np.arange(S_LEN, dtype=np.float32)
    freqs = (10000.0 ** (-np.arange(0, 32, 2, dtype=np.float32) / 32)).astype(np.float32)
    ang = (pos[:, None] * freqs[None, :]).astype(np.float32)
    cos, sin = np.cos(ang).astype(np.float32), np.sin(ang).astype(np.float32)
    cs = np.concatenate([cos, cos], 1)
    sg = np.concatenate([-sin, sin], 1)
    cst["cs_t"] = np.ascontiguousarray(cs.reshape(NT, 128, 32).transpose(1, 0, 2))
    cst["sg_t"] = np.ascontiguousarray(sg.reshape(NT, 128, 32).transpose(1, 0, 2))
    ki = np.arange(128)[:, None]
    qi = np.arange(128)[None, :]
    mt = np.zeros((128, 23, 128), np.float32)
    for jj in range(23):
        j = jj - 3
        d = 128 * j + qi - ki
        m = ((d >= 0) & (d <= 128)).astype(np.float32) + ((d >= 0) & (d % 4 == 0) & (d <= 512)) \
            + ((d >= 0) & (d % 16 == 0) & (d <= 2048))
        mt[:, jj, :] = m
    cst["mtab"] = mt.reshape(128, 23 * 128).astype(bf)
    ct = np.zeros((128, 7, 128), np.float32)
    for jj in range(7):
        j = jj - 3
        d = 128 * j + qi - ki
        ct[:, jj, :] = (d >= 0)
    cst["ctab"] = ct.reshape(128, 7 * 128).astype(bf)
    cst["negm"] = np.where(np.arange(128)[None, :] <= np.arange(128)[:, None], 0.0, -1e30).astype(np.float32)
    cst["halfpow"] = np.tile((0.5 ** np.arange(1, 17)).astype(np.float32)[None, :], (128, 1))
    kpos = np.zeros((N_ALIBI, 32, S_LEN), np.float32)
    qpos = np.zeros((N_ALIBI, 32, S_LEN), np.float32)
    tok = np.arange(S_LEN)
    u = (tok % 128 - 64).astype(np.float32)
    tt = (tok // 128).astype(np.float32)
    for h in range(N_ALIBI):
        c1 = np.float32(np.float32(SLOPES[h]).astype(bf))
        c2 = np.float32(np.float32(np.float32(SLOPES[h]) - c1).astype(bf))
        kpos[h, 0:6] = np.stack([u, u, tt, tt, np.full(S_LEN, c1), np.full(S_LEN, c2)])
        qpos[h, 0:6] = np.stack([np.full(S_LEN, c1), np.full(S_LEN, c2), np.full(S_LEN, 128 * c1),
                                 np.full(S_LEN, 128 * c2), -128 * tt, -128 * tt])
    cst["kpos"] = kpos.astype(bf)
    cst["qpos"] = qpos.astype(bf)
    return cst


def _prep_shared(w_ada, b_ada, w_in, q_norm_g, kv_norm_g, w_uq, w_uk, w_uv, w_out, ln_g, ln_b):
    L = w_in.shape[0]
    sh = {}
    sh["w_ada"] = np.ascontiguousarray(w_ada, dtype=np.float32)
    sh["b_ada"] = np.ascontiguousarray(b_ada, dtype=np.float32).reshape(L, 1, 3 * DM)
    kr = w_in[:, :, 1024:1056]
    krs = np.concatenate([kr[:, :, 16:32], kr[:, :, 0:16]], axis=2)
    sh["w_in"] = np.ascontiguousarray(np.concatenate([w_in[:, :, :1056], krs, w_in[:, :, 1056:]], axis=2),
                                      dtype=np.float32)
    uq = w_uq.reshape(L, 768, 6, 96)
    rope = uq[..., 64:96]
    rope_s = np.concatenate([rope[..., 16:32], rope[..., 0:16]], axis=-1)
    sh["w_uq"] = np.ascontiguousarray(np.concatenate([uq, rope_s], axis=-1).reshape(L, 768, 768), dtype=np.float32)
    sh["w_ukv"] = np.ascontiguousarray(np.concatenate([w_uk, w_uv], axis=2), dtype=np.float32)
    sh["w_out"] = np.ascontiguousarray(w_out, dtype=np.float32)
    sh["qg_col"] = np.ascontiguousarray(q_norm_g.reshape(L, 6, 128).transpose(0, 2, 1), dtype=np.float32)
    sh["kvg_col"] = np.ascontiguousarray(kv_norm_g.reshape(L, 2, 128).transpose(0, 2, 1), dtype=np.float32)
    sh["lng_b"] = np.ascontiguousarray(np.broadcast_to(ln_g[:, None, :], (L, 128, DM)), dtype=np.float32)
    sh["lnb_b"] = np.ascontiguousarray(np.broadcast_to(ln_b[:, None, :], (L, 128, DM)), dtype=np.float32)
    return sh


_NC_CACHE = {}


def kernel(x, c, w_ada, b_ada, w_in, q_norm_g, kv_norm_g, w_uq, w_uk, w_uv, w_out, ln_g, ln_b):
    x = np.asarray(x, dtype=np.float32)
    c = np.asarray(c, dtype=np.float32)
    args = [np.asarray(a, dtype=np.float32) for a in
            (w_ada, b_ada, w_in, q_norm_g, kv_norm_g, w_uq, w_uk, w_uv, w_out, ln_g, ln_b)]
    shared = _prep_shared(*args)
    shared.update(_consts())
    if "nc" not in _NC_CACHE:
        _NC_CACHE["nc"] = build_nc()[0]
    nc = _NC_CACHE["nc"]
    B = x.shape[0]
    in_maps = []
    for b in range(B):
        m = dict(shared)
        m["x"] = np.ascontiguousarray(x[b])
        m["c_col"] = np.ascontiguousarray(c[b].reshape(8, 128).T)
        in_maps.append(m)
    res = run_bass_kernel_spmd(nc, in_maps, core_ids=list(range(B)))
    return np.stack([np.asarray(r["out"], dtype=np.float32) for r in res.results], axis=0)
```

```python
import math
from contextlib import ExitStack
import numpy as np
import ml_dtypes
import concourse.bass as bass
import concourse.mybir as mybir
from concourse.bass_utils import run_bass_kernel_spmd

F32 = mybir.dt.float32
BF16 = mybir.dt.bfloat16
AF = mybir.ActivationFunctionType
ALU = mybir.AluOpType
AX = mybir.AxisListType

ENGS = ("pe", "act", "dve", "pool", "sp")
DMA_RING = {"sp": 40, "pool": 24, "act": 16}
SIG_CHUNK = 16384


class Buf:
    _n = 0

    def __init__(self, name="b"):
        Buf._n += 1
        self.name = f"{name}#{Buf._n}"

    def __getitem__(self, key):
        return (self, key)


def _norm(x):
    if isinstance(x, Buf):
        return (x, None)
    if isinstance(x, Tile):
        return (x.buf, None)
    return x


class Tile:
    def __init__(self, ap, name="t"):
        self.ap = ap
        self.buf = Buf(name)

    def __getitem__(self, key):
        return self.ap[key]

    def k(self, key):
        return (self.buf, key)


class Op:
    __slots__ = ("eng", "fn", "reads", "writes", "dma", "idx", "deps", "sig", "sigidx",
                 "dsem", "dval", "dprev", "barrier", "cons")

    def __init__(self, eng, fn, reads, writes, dma):
        self.eng = eng
        self.fn = fn
        self.reads = [_norm(r) for r in reads]
        self.writes = [_norm(w) for w in writes]
        self.dma = dma
        self.deps = []
        self.sig = False
        self.sigidx = None
        self.barrier = False
        self.cons = None


class Sched:
    def __init__(self, nc):
        self.nc = nc
        self.ops = []

    def add(self, eng, fn, reads=(), writes=(), dma=False):
        op = Op(eng, fn, reads, writes, dma)
        op.idx = len(self.ops)
        self.ops.append(op)
        return op

    def pe(self, fn, reads=(), writes=()):
        return self.add("pe", fn, reads, writes)

    def act(self, fn, reads=(), writes=()):
        return self.add("act", fn, reads, writes)

    def dve(self, fn, reads=(), writes=()):
        return self.add("dve", fn, reads, writes)

    def pool(self, fn, reads=(), writes=()):
        return self.add("pool", fn, reads, writes)

    def dma(self, out, in_, reads=(), writes=(), q="sp", **kw):
        return self.add(q, lambda e: e.dma_start(out=out, in_=in_, **kw), reads, writes, dma=True)

    def barrier(self):
        h = self.add("dve", None, (), ())
        h.barrier = "hub"
        scr = self.scr
        r1 = self.add("dve", lambda e: e.memset(scr, 0.0), (), ())
        r2 = self.add("dve", lambda e: e.memset(scr, 0.0), (), ())
        for e, r in (("pe", r1), ("act", r1), ("pool", r2), ("sp", r2)):
            b = self.add(e, None, (), ())
            b.barrier = ("wait", r)

    def analyze(self):
        last_w = {}
        readers = {}
        last_on_eng = {e: None for e in ENGS}
        pending_dma = []
        for op in self.ops:
            deps = []
            if op.barrier == "hub":
                for e in ENGS:
                    if last_on_eng[e] is not None and e != op.eng:
                        deps.append((last_on_eng[e], "bar"))
                for d in pending_dma:
                    deps.append((d, "bar"))
                pending_dma = []
                last_w.clear()
                readers.clear()
            elif op.barrier:
                deps.append((op.barrier[1], "bar"))
            else:
                op_nr = getattr(op.fn, "_nr", ())
                def conflicts(table, buf, key):
                    ent = table.get(buf)
                    if not ent:
                        return
                    if key is None:
                        for v in ent.values():
                            yield v
                    else:
                        if key in ent:
                            yield ent[key]
                        if None in ent:
                            yield ent[None]
                for (b, k) in op.reads:
                    for w in conflicts(last_w, b, k):
                        deps.append((w, "raw"))
                for (b, k) in op.writes:
                    for w in conflicts(last_w, b, k):
                        deps.append((w, "waw"))
                    for rl in conflicts(readers, b, k):
                        for r in rl.values():
                            deps.append((r, "war"))
                for (b, k) in op.writes:
                    ent = last_w.setdefault(b, {})
                    rent = readers.setdefault(b, {})
                    if k is None:
                        ent.clear()
                        rent.clear()
                    else:
                        rent.pop(k, None)
                    ent[k] = op
                for (b, k) in op.reads:
                    if (b, k) in op_nr:
                        continue
                    rd = readers.setdefault(b, {}).setdefault(k, {})
                    rd[("dma", op.idx) if op.dma else op.eng] = op
            fdeps = []
            seen = set()
            for p, kind in deps:
                if p is op:
                    continue
                if (not p.dma) and (not op.dma) and p.eng == op.eng:
                    if op.eng == "pe" or kind != "raw":
                        continue
                if p.idx in seen:
                    continue
                seen.add(p.idx)
                fdeps.append(p)
            op.deps = fdeps
            for p in fdeps:
                if not p.dma:
                    p.sig = True
                    if p.cons is None:
                        p.cons = set()
                    p.cons.add(op.eng)
            if op.dma:
                pending_dma.append(op)
            elif not op.barrier:
                last_on_eng[op.eng] = op

    def emit(self):
        nc = self.nc
        self.analyze()
        sigcnt = {}
        dcnt = {q: 0 for q in DMA_RING}
        for op in self.ops:
            if op.dma:
                n = DMA_RING[op.eng]
                c = dcnt[op.eng]
                op.dsem = (op.eng, c % n)
                op.dval = 16 * (c // n + 1)
                op.dprev = 16 * (c // n)
                dcnt[op.eng] += 1
            elif op.sig:
                op.sigidx = {}
                for ce in sorted(op.cons):
                    key = (op.eng, ce)
                    op.sigidx[ce] = sigcnt.get(key, 0)
                    sigcnt[key] = sigcnt.get(key, 0) + 1
        with ExitStack() as st:
            esems = {}
            for key, cntv in sigcnt.items():
                n = cntv // SIG_CHUNK + 1
                esems[key] = [st.enter_context(nc.semaphore(f"s_{key[0]}_{key[1]}_{i}")) for i in range(n)]
            dsems = {}
            for q, n in DMA_RING.items():
                for i in range(min(n, dcnt[q])):
                    dsems[(q, i)] = st.enter_context(nc.semaphore(f"s_dma_{q}_{i}"))
            block = st.enter_context(nc.Block())
            per_eng = {e: [op for op in self.ops if op.eng == e] for e in ENGS}

            def run(eng_name, eng):
                waited = {}

                def wait(key, sem, val):
                    if waited.get(key, 0) >= val:
                        return
                    waited[key] = val
                    eng.wait_ge(sem, val)

                for op in per_eng[eng_name]:
                    for p in op.deps:
                        if p.dma:
                            wait(("d", p.dsem), dsems[p.dsem], p.dval)
                        else:
                            c, v = divmod(p.sigidx[eng_name], SIG_CHUNK)
                            wait((p.eng, c), esems[(p.eng, eng_name)][c], v + 1)
                    if op.dma and op.dprev > 0:
                        wait(("d", op.dsem), dsems[op.dsem], op.dprev)
                    if op.barrier:
                        continue
                    ins = op.fn(eng)
                    if op.dma:
                        ins.then_inc(dsems[op.dsem], 16)
                    elif op.sig:
                        items = list(op.sigidx.items())
                        for n_, (ce, si) in enumerate(items):
                            if n_ >= 1:
                                if eng_name == "pe":
                                    assert getattr(op.fn, "_idem", False), f"non-idempotent PE op with consumers {items}"
                                    ins = op.fn(eng)
                                elif eng_name == "act":
                                    ins = eng.activation(out=self.scr_act, in_=self.scr_act, func=AF.Copy)
                                else:
                                    ins = eng.memset(self.scr_pool if eng_name == "pool" else self.scr, 0.0)
                            c, v = divmod(si, SIG_CHUNK)
                            ins.then_inc(esems[(op.eng, ce)][c], 1)

            @block.tensor
            def _(eng):
                run("pe", eng)

            @block.scalar
            def _(eng):
                run("act", eng)

            @block.vector
            def _(eng):
                run("dve", eng)

            @block.gpsimd
            def _(eng):
                run("pool", eng)

            @block.sync
            def _(eng):
                run("sp", eng)
        return sigcnt, dcnt


class SBAlloc:
    def __init__(self, nc, nwords):
        self.t = nc.alloc_sbuf_tensor("sbig", [128, nwords], F32)
        self.n = nwords
        self.off = 0

    def mark(self):
        return self.off

    def release(self, m):
        self.off = m

    def alloc(self, shape, dtype, name="t"):
        nelem = 1
        for s in shape[1:]:
            nelem *= s
        nbytes = nelem * (4 if dtype == F32 else 2)
        words = (nbytes + 3) // 4
        words = (words + 7) // 8 * 8
        assert self.off + words <= self.n, f"SBUF overflow allocating {name} {shape}: {self.off}+{words}>{self.n}"
        ap = self.t[0:shape[0], self.off:self.off + words]
        self.off += words
        if dtype != F32:
            ap = ap.bitcast(dtype)
        ap = ap[:, 0:nelem]
        if len(shape) == 3:
            ap = ap.rearrange("p (a b) -> p a b", a=shape[1], b=shape[2])
        elif len(shape) == 4:
            ap = ap.rearrange("p (a b c) -> p a b c", a=shape[1], b=shape[2], c=shape[3])
        return Tile(ap, name)


DEPTH = 2
S_LEN = 4096
DM = 1024
NT = 32
NG = 8
ALPHA = (2 * DEPTH) ** 0.25
INW = 4616
PG = [(0, 512), (512, 512), (1024, 448), (1472, 384), (1856, 384), (2240, 512), (2752, 512), (3264, 328),
      (3592, 512), (4104, 512)]
N_ALIBI = 10
SLOPES = [2.0 ** (-8.0 * (i + 1) / N_ALIBI) for i in range(N_ALIBI)]


PSTOP = 99
NGLIM = 8
EVMODE = 2


def build_nc(depth=DEPTH, debug=False, phases="PABCO"):
    nc = bass.Bass("TRN2", target_bir_lowering=False)
    S = Sched(nc)

    def din(name, shape, dt=F32):
        return nc.dram_tensor(name, list(shape), dt, kind="ExternalInput").ap()

    def dscr(name, shape, dt=BF16):
        if debug:
            return nc.dram_tensor(name, list(shape), dt, kind="ExternalOutput").ap()
        return nc.dram_tensor(name, list(shape), dt).ap()

    x_in = din("x", [S_LEN, DM])
    c_col = din("c_col", [128, 8])
    w_ada = din("w_ada", [depth, DM, 3 * DM])
    b_ada = din("b_ada", [depth, 1, 3 * DM])
    w_in = din("w_in", [depth, DM, INW])
    w_uq = din("w_uq", [depth, 768, 768])
    w_ukv = din("w_ukv", [depth, 256, 768])
    w_out = din("w_out", [depth, DM, DM])
    qg_col = din("qg_col", [depth, 128, 6])
    kvg_col = din("kvg_col", [depth, 128, 2])
    lng_b = din("lng_b", [depth, 128, DM])
    lnb_b = din("lnb_b", [depth, 128, DM])
    ident_f_d = din("ident_f", [128, 128])
    ident_b_d = din("ident_b", [128, 128], BF16)
    cs_d = din("cs_t", [128, NT, 32])
    sg_d = din("sg_t", [128, NT, 32])
    mtab_d = din("mtab", [128, 23 * 128], BF16)
    ctab_d = din("ctab", [128, 7 * 128], BF16)
    negm_d = din("negm", [128, 128])
    hp_d = din("halfpow", [128, 16])
    kpos_d = din("kpos", [N_ALIBI, 32, S_LEN], BF16)
    qpos_d = din("qpos", [N_ALIBI, 32, S_LEN], BF16)
    out_d = nc.dram_tensor("out", [S_LEN, DM], F32, kind="ExternalOutput").ap()

    QaT_d = dscr("QaT_d", [96, 6, S_LEN]); KaT_d = dscr("KaT_d", [96, 6, S_LEN])
    Va_d = dscr("Va_d", [S_LEN, 6 * 65])
    QbT_d = dscr("QbT_d", [64, 6, S_LEN]); KbT_d = dscr("KbT_d", [64, 6, S_LEN])
    Vb_d = dscr("Vb_d", [S_LEN, 6 * 65])
    QcT_d = dscr("QcT_d", [64, 4, S_LEN]); KcT_d = dscr("KcT_d", [64, 4, S_LEN])
    Vc_d = dscr("Vc_d", [S_LEN, 4 * 65])
    QiT_d = dscr("QiT_d", [128, 4, S_LEN]); KiT_d = dscr("KiT_d", [128, S_LEN])
    sgn_d = dscr("sgn_d", [S_LEN, 8], F32)
    gT_d = dscr("gT_d", [128, 8, S_LEN])
    yT_d = dscr("yT_d", [DM, S_LEN])
    xmid_d = dscr("xmid_d", [S_LEN, DM], F32)
    d_bufs = {n: Buf(n) for n in ["QaT", "KaT", "Va", "QbT", "KbT", "Vb", "QcT", "KcT", "Vc", "QiT", "KiT",
                                  "sgn", "gT", "yT", "xmid", "out"]}

    sb = SBAlloc(nc, 51500)
    ps = nc.alloc_psum_tensor("psum", [128, 4096], F32)
    S.scr = sb.alloc([1, 8], F32, "scr_dve").ap
    S.scr_act = sb.alloc([1, 8], F32, "scr_act").ap
    S.scr_pool = sb.alloc([1, 8], F32, "scr_pool").ap
    pbuf = [Buf(f"bank{i}") for i in range(8)]

    def bank(i):
        return ps[:, i * 512:(i + 1) * 512]

    def bank_bf(i):
        return ps[:, i * 512:(i + 1) * 512].bitcast(BF16)

    ident_f = sb.alloc([128, 128], F32, "ident_f")
    ident_b = sb.alloc([128, 128], BF16, "ident_b")
    cs_t = sb.alloc([128, NT, 32], F32, "cs")
    sg_t = sb.alloc([128, NT, 32], F32, "sg")
    mtab = sb.alloc([128, 23 * 128], BF16, "mtab")
    ctab = sb.alloc([128, 7 * 128], BF16, "ctab")
    negm = sb.alloc([128, 128], F32, "negm")
    halfpow = sb.alloc([128, 16], F32, "hp")
    ones_f = sb.alloc([128, 128], F32, "ones")
    ones_b = sb.alloc([128, 64], BF16, "ones_b")
    silu_c = sb.alloc([128, 8], F32, "siluc")
    c_sb = sb.alloc([128, 8], F32, "c")
    sc_col = sb.alloc([128, 8], F32, "sc_col")
    sh_col = sb.alloc([128, 8], F32, "sh_col")
    gate_b = sb.alloc([128, DM], F32, "gate_b")
    for t, d in [(ident_f, ident_f_d), (ident_b, ident_b_d), (cs_t, cs_d), (sg_t, sg_d), (mtab, mtab_d),
                 (ctab, ctab_d), (negm, negm_d), (halfpow, hp_d), (c_sb, c_col)]:
        S.dma(t.ap, d, writes=[t])
    S.dve(lambda e: e.memset(ones_f.ap, 1.0), writes=[ones_f])
    S.dve(lambda e: e.memset(S.scr, 0.0), writes=[Buf("scr")])
    S.dve(lambda e: e.memset(S.scr_act, 0.0), writes=[Buf("scra")])
    S.dve(lambda e: e.memset(S.scr_pool, 0.0), writes=[Buf("scrp")])
    S.dve(lambda e: e.memset(ones_b.ap, 1.0), writes=[ones_b])
    S.act(lambda e: e.activation(out=silu_c.ap, in_=c_sb.ap, func=AF.Silu), reads=[c_sb], writes=[silu_c])

    def mm(out, lhsT, rhs, start, stop, reads, writes, nr=()):
        o = S.pe(lambda e: e.matmul(out, lhsT=lhsT, rhs=rhs, start=start, stop=stop), reads, writes)
        o.fn._idem = bool(start and stop)
        if nr:
            o.fn._nr = tuple(_norm(x) for x in nr)
        return o

    def tr(out, in_, ident, reads, writes):
        o = S.pe(lambda e: e.transpose(out, in_, ident), reads, writes)
        o.fn._idem = True
        return o

    for l in range(depth):
        x_src = x_in if l == 0 else xmid_d
        x_src_buf = None if l == 0 else d_bufs["xmid"]
        x_dst = out_d if l == depth - 1 else xmid_d
        x_dst_buf = d_bufs["out"] if l == depth - 1 else d_bufs["xmid"]
        xr = [x_src_buf] if x_src_buf is not None else []

        m0 = sb.mark()
        wa = [sb.alloc([128, 3 * DM], F32, f"wa{i}") for i in range(2)]
        bada = sb.alloc([1, 3 * DM], F32, "bada")
        mod_row = sb.alloc([1, 3 * DM], F32, "mod_row")
        S.dma(bada.ap, b_ada[l], writes=[bada])
        for k in range(8):
            w_ = wa[k % 2]
            S.dma(w_.ap, w_ada[l, k * 128:(k + 1) * 128, :], writes=[w_])
            for cg in range(6):
                mm(bank(cg)[0:1, :], silu_c[:, k:k + 1], w_[:, cg * 512:(cg + 1) * 512], k == 0, k == 7,
                   [silu_c, w_], [pbuf[cg]])
        for cg in range(6):
            S.dve(lambda e, cg=cg: e.tensor_tensor(out=mod_row[0:1, cg * 512:(cg + 1) * 512], in0=bank(cg)[0:1, :],
                                                   in1=bada[0:1, cg * 512:(cg + 1) * 512], op=ALU.add),
                  reads=[pbuf[cg], bada], writes=[mod_row.k(cg)])
        for j in range(16):
            mm(bank(6)[:, 2 * j:2 * j + 2], mod_row[0:1, j * 128:(j + 1) * 128], ones_f[0:1, 0:2], True, True,
               [mod_row, ones_f], [pbuf[6]])
        colv = bank(6)[:, 0:32].rearrange("p (a b) -> p a b", b=2)
        S.dve(lambda e: e.tensor_copy(out=sh_col.ap, in_=colv[:, 0:8, 0]), reads=[pbuf[6]], writes=[sh_col])
        S.dve(lambda e: e.tensor_scalar(out=sc_col.ap, in0=colv[:, 8:16, 0], scalar1=1.0, scalar2=None, op0=ALU.add),
              reads=[pbuf[6]], writes=[sc_col])
        for cg in range(2):
            mm(bank(7), ones_f[0:1, 0:128], mod_row[0:1, 2048 + cg * 512:2048 + (cg + 1) * 512], True, True,
               [mod_row, ones_f], [pbuf[7]])
            S.dve(lambda e, cg=cg: e.tensor_scalar(out=gate_b[:, cg * 512:(cg + 1) * 512], in0=bank(7), scalar1=1.0,
                                                   scalar2=None, op0=ALU.add),
                  reads=[pbuf[7]], writes=[gate_b.k(cg)])
        S.barrier()
        sb.release(m0)

        if "P" in phases:
            m0 = sb.mark()
            w_in_sb = sb.alloc([128, 8, INW], BF16, "w_in")
            for k in range(8):
                for c3 in range(4):
                    c0, c1 = c3 * 1154, (c3 + 1) * 1154
                    S.dma(w_in_sb[:, k, c0:c1], w_in[l, k * 128:(k + 1) * 128, c0:c1], writes=[w_in_sb.k((k, c3))],
                          q="pool")
            wuq_sb = sb.alloc([128, 6, 768], BF16, "wuq")
            wukv_sb = sb.alloc([128, 2, 768], BF16, "wukv")
            qg_sb = sb.alloc([128, 6], F32, "qg")
            kvg_sb = sb.alloc([128, 2], F32, "kvg")
            S.dma(qg_sb.ap, qg_col[l], writes=[qg_sb])
            S.dma(kvg_sb.ap, kvg_col[l], writes=[kvg_sb])
            m1 = sb.mark()
            wst = sb.alloc([128, 8, 768], F32, "wst")
            S.dma(wst[:, 0:6, :], w_uq[l].rearrange("(k p) n -> p k n", p=128), writes=[wst.k("q")])
            S.dma(wst[:, 6:8, :], w_ukv[l].rearrange("(k p) n -> p k n", p=128), writes=[wst.k("kv")])
            for k in range(6):
                S.dve(lambda e, k=k: e.tensor_scalar(out=wuq_sb[:, k, :], in0=wst[:, k, :], scalar1=qg_sb[:, k:k + 1],
                                                     scalar2=None, op0=ALU.mult),
                      reads=[wst.k("q"), qg_sb], writes=[wuq_sb.k(k)])
            for k in range(2):
                S.dve(lambda e, k=k: e.tensor_scalar(out=wukv_sb[:, k, :], in0=wst[:, 6 + k, :],
                                                     scalar1=kvg_sb[:, k:k + 1], scalar2=None, op0=ALU.mult),
                      reads=[wst.k("kv"), kvg_sb], writes=[wukv_sb.k(k)])
            S.barrier()
            sb.release(m1)

            xt = [sb.alloc([128, DM], F32, f"xt{i}") for i in range(2)]
            hT = [sb.alloc([128, 8, 128], BF16, f"hT{i}") for i in range(2)]
            cq_bf = sb.alloc([128, 1024], BF16, "cq_bf")
            cT = sb.alloc([128, 8, 128], BF16, "cT")
            ssq = sb.alloc([128, 4], F32, "ssq")
            rv = sb.alloc([128, 2], F32, "rv")
            rr = sb.alloc([128, 2], F32, "rr")
            junk = sb.alloc([128, 512], F32, "junkP")
            krt = sb.alloc([128, 3, 32], F32, "krt")
            krope = sb.alloc([128, 32], BF16, "krope")
            qb_bf = sb.alloc([128, 384], BF16, "qb_bf")
            kb_bf = sb.alloc([128, 384], BF16, "kb_bf")
            qc_bf = sb.alloc([128, 256], BF16, "qc_bf")
            kc_bf = sb.alloc([128, 256], BF16, "kc_bf")
            qi_f = sb.alloc([128, 8, 64], F32, "qi_f")
            qi_bf = sb.alloc([128, 8, 64], BF16, "qi_bf")
            ki_dup = sb.alloc([128, 2, 64], BF16, "ki_dup")
            absw = sb.alloc([128, 8], F32, "absw")
            gate_bf = sb.alloc([128, DM], BF16, "gate_bf")
            qa_f = sb.alloc([128, 6, 128], F32, "qa_f")
            tmr = sb.alloc([128, 2, 6, 32], F32, "tmr")
            qA = sb.alloc([128, 6, 96], BF16, "qA")
            kA = sb.alloc([128, 6, 96], BF16, "kA")
            QaT_st = sb.alloc([96, 6, 512], BF16, "QaT_st"); KaT_st = sb.alloc([96, 6, 512], BF16, "KaT_st")
            QbT_st = sb.alloc([64, 6, 512], BF16, "QbT_st"); KbT_st = sb.alloc([64, 6, 512], BF16, "KbT_st")
            QcT_st = sb.alloc([64, 4, 512], BF16, "QcT_st"); KcT_st = sb.alloc([64, 4, 512], BF16, "KcT_st")
            QiT_st = sb.alloc([128, 4, 512], BF16, "QiT_st"); KiT_st = sb.alloc([128, 512], BF16, "KiT_st")
            gT_st = sb.alloc([128, 8, 512], BF16, "gT_st")
            Va_st = sb.alloc([128, 4, 6, 65], BF16, "Va_st"); Vb_st = sb.alloc([128, 4, 6, 65], BF16, "Vb_st")
            Vc_st = sb.alloc([128, 4, 4, 65], BF16, "Vc_st")
            sgn_st = sb.alloc([128, 4, 8], F32, "sgn_st")
            for vs in (Va_st, Vb_st, Vc_st):
                S.pool(lambda e, vs=vs: e.memset(vs.ap, 1.0), writes=[vs])

            def evac_copy(eng, out, in_, reads, writes, scale=None):
                if eng == "act":
                    if scale is None:
                        S.act(lambda e: e.activation(out=out, in_=in_, func=AF.Copy), reads, writes)
                    else:
                        S.act(lambda e: e.activation(out=out, in_=in_, func=AF.Copy, scale=scale), reads, writes)
                else:
                    if scale is None:
                        S.dve(lambda e: e.tensor_copy(out=out, in_=in_), reads, writes)
                    else:
                        S.dve(lambda e: e.tensor_scalar(out=out, in0=in_, scalar1=scale, scalar2=None, op0=ALU.mult),
                              reads, writes)

            trc = [0]

            def tgroup(srcs, src_reads, width, dst, dst_w, eng):
                b = (7, 0, 1)[trc[0] % 3]
                n = len(srcs)
                pv = bank_bf(b)
                for j, s_ap in enumerate(srcs):
                    tr(pv[0:width, j * 128:(j + 1) * 128], s_ap, ident_b.ap, list(src_reads) + [ident_b], [pbuf[b]])
                src = pv[0:width, 0:n * 128]
                if n > 1:
                    src = src.rearrange("p (a b) -> p a b", a=n, b=128)
                evac_copy(eng, dst, src, [pbuf[b]], dst_w)
                trc[0] += 1

            for g in range(min(NG, NGLIM) if PSTOP > 1 else 0):
                for i in range(4):
                    t = 4 * g + i
                    tsl = slice(i * 128, (i + 1) * 128)
                    xb = xt[t % 2]
                    hb = hT[t % 2]
                    S.dma(xb.ap, x_src[t * 128:(t + 1) * 128, :], reads=xr, writes=[xb])
                    for k in range(8):
                        bk = k // 4
                        o_ = tr(bank(bk)[:, (k % 4) * 128:(k % 4 + 1) * 128], xb[:, k * 128:(k + 1) * 128], ident_f.ap,
                                [xb, ident_f], [pbuf[bk]])
                        o_.fn._nr = ((xb.buf, None),)
                    for k in range(8):
                        bk = k // 4
                        src = bank(bk)[:, (k % 4) * 128:(k % 4 + 1) * 128]
                        if (k % 2 == 0 and EVMODE == 0) or EVMODE == 1 or (EVMODE == 3 and k < 4):
                            S.act(lambda e, k=k, src=src, hb=hb: e.activation(out=hb[:, k, :], in_=src, func=AF.Identity,
                                                                             scale=sc_col[:, k:k + 1],
                                                                             bias=sh_col[:, k:k + 1]),
                                  reads=[pbuf[bk], sc_col, sh_col, xb], writes=[hb.k(k)])
                        else:
                            S.dve(lambda e, k=k, src=src, hb=hb: e.tensor_scalar(out=hb[:, k, :], in0=src,
                                                                                scalar1=sc_col[:, k:k + 1],
                                                                                scalar2=sh_col[:, k:k + 1],
                                                                                op0=ALU.mult, op1=ALU.add),
                                  reads=[pbuf[bk], sc_col, sh_col, xb], writes=[hb.k(k)])
                    if PSTOP <= 2:
                        continue
                    for gi, (c0, cw) in enumerate(PG):
                        b = 2 + gi % 3
                        pb = bank(b)
                        for k in range(8):
                            mm(pb[:, 0:cw], hb[:, k, :], w_in_sb[:, k, c0:c0 + cw], k == 0, k == 7,
                               [hb, w_in_sb], [pbuf[b]], nr=[hb])
                        R = [pbuf[b]]
                        if gi == 0:
                            evac_copy("act", cq_bf[:, 0:512], pb, R, [cq_bf.k(0)])
                            S.act(lambda e: e.activation(out=junk.ap, in_=cq_bf[:, 0:512], func=AF.Square,
                                                         accum_out=ssq[:, 0:1]), [cq_bf.k(0)], [junk, ssq.k(0)])
                        elif gi == 1:
                            evac_copy("act", cq_bf[:, 512:1024], pb, R, [cq_bf.k(1)])
                            S.act(lambda e: e.activation(out=junk[:, 0:256], in_=cq_bf[:, 512:768], func=AF.Square,
                                                         accum_out=ssq[:, 1:2]), [cq_bf.k(1)], [junk, ssq.k(1)])
                            S.act(lambda e: e.activation(out=junk[:, 256:512], in_=cq_bf[:, 768:1024],
                                                         func=AF.Square, accum_out=ssq[:, 2:3]),
                                  [cq_bf.k(1)], [junk, ssq.k(2)])
                        elif gi == 2:
                            evac_copy("act", krt[:, 2, :], pb[:, 0:32], R, [krt.k(2)])
                            evac_copy("act", krt[:, 1, :], pb[:, 32:64], R, [krt.k(1)])
                            S.dve(lambda e, t=t: e.tensor_tensor(out=krt[:, 0, :], in0=krt[:, 2, :],
                                                                 in1=cs_t[:, t, :], op=ALU.mult),
                                  [krt.k(2), cs_t], [krt.k(0)])
                            S.dve(lambda e, t=t: e.tensor_tensor(out=krt[:, 1, :], in0=krt[:, 1, :],
                                                                 in1=sg_t[:, t, :], op=ALU.mult),
                                  [krt.k(1), sg_t], [krt.k(1)])
                            S.dve(lambda e: e.tensor_tensor(out=krope.ap, in0=krt[:, 0, :], in1=krt[:, 1, :], op=ALU.add),
                                  [krt], [krope])
                            evac_copy("act", qb_bf.ap, pb[:, 64:448], R, [qb_bf], scale=0.125)
                        elif gi == 3:
                            evac_copy("dve", kb_bf.ap, pb[:, 0:384], R, [kb_bf])
                        elif gi == 4:
                            evac_copy("dve", Vb_st[:, i, :, 0:64],
                                      pb[:, 0:384].rearrange("p (a b) -> p a b", a=6, b=64), R, [Vb_st.k(i)])
                        elif gi == 5:
                            evac_copy("dve", qc_bf.ap, pb[:, 0:256], R, [qc_bf], scale=0.125)
                            evac_copy("dve", kc_bf.ap, pb[:, 256:512], R, [kc_bf])
                        elif gi == 6:
                            evac_copy("act", Vc_st[:, i, :, 0:64],
                                      pb[:, 0:256].rearrange("p (a b) -> p a b", a=4, b=64), R, [Vc_st.k(i)])
                            evac_copy("act", qi_f[:, 0:4, :], pb[:, 256:512].rearrange("p (a b) -> p a b", a=4, b=64),
                                      R, [qi_f.k(0)])
                        elif gi == 7:
                            evac_copy("act", qi_f[:, 4:8, :], pb[:, 0:256].rearrange("p (a b) -> p a b", a=4, b=64),
                                      R, [qi_f.k(1)])
                            evac_copy("act", ki_dup[:, 0, :], pb[:, 256:320], R, [ki_dup.k(0)])
                            evac_copy("act", ki_dup[:, 1, :], pb[:, 256:320], R, [ki_dup.k(1)])
                            S.act(lambda e, pb=pb: e.activation(out=absw.ap, in_=pb[:, 320:328], func=AF.Abs,
                                                                scale=1.0 / (8.0 * math.sqrt(8.0))), R, [absw])
                            S.act(lambda e, pb=pb, i=i: e.activation(out=sgn_st[:, i, :], in_=pb[:, 320:328],
                                                                     func=AF.Sign), R, [sgn_st.k(i)])
                            S.dve(lambda e: e.tensor_tensor(out=qi_bf.ap, in0=qi_f.ap,
                                                            in1=absw.ap.unsqueeze(2).to_broadcast([128, 8, 64]),
                                                            op=ALU.mult), [qi_f, absw], [qi_bf])
                        else:
                            h0 = (gi - 8) * 512
                            S.act(lambda e, pb=pb, h0=h0: e.activation(out=gate_bf[:, h0:h0 + 512], in_=pb,
                                                                       func=AF.Silu), R + [hb], [gate_bf.k(gi)])
                    if PSTOP <= 3:
                        continue
                    S.dve(lambda e: e.tensor_tensor(out=rv[:, 0:1], in0=ssq[:, 0:1], in1=ssq[:, 1:2], op=ALU.add),
                          [ssq], [rv.k(0)])
                    S.dve(lambda e: e.tensor_scalar(out=rv[:, 0:1], in0=rv[:, 0:1], scalar1=1.0 / 8.0, scalar2=96e-6,
                                                    op0=ALU.mult, op1=ALU.add), [rv.k(0)], [rv.k(0)])
                    S.dve(lambda e: e.tensor_scalar(out=rv[:, 1:2], in0=ssq[:, 2:3], scalar1=1.0 / 256.0, scalar2=1e-6,
                                                    op0=ALU.mult, op1=ALU.add), [ssq], [rv.k(1)])
                    S.act(lambda e: e.activation(out=rr.ap, in_=rv.ap, func=AF.Sqrt), [rv], [rr])
                    S.dve(lambda e: e.reciprocal(out=rr.ap, in_=rr.ap), [rr], [rr])
                    tgroup([cq_bf[:, k * 128:(k + 1) * 128] for k in range(8)], [cq_bf], 128, cT.ap, [cT], "act")
                    for cg in range(2):
                        b = 5 + cg
                        for k in range(6):
                            mm(bank(b)[:, 0:384], cT[:, k, :], wuq_sb[:, k, cg * 384:(cg + 1) * 384], k == 0, k == 5,
                               [cT, wuq_sb], [pbuf[b]])
                        S.act(lambda e, b=b, cg=cg: e.activation(
                            out=qa_f[:, cg * 3:(cg + 1) * 3, :],
                            in_=bank(b)[:, 0:384].rearrange("p (a b) -> p a b", a=3, b=128),
                            func=AF.Identity, scale=rr[:, 0:1]), [pbuf[b], rr], [qa_f.k(cg)])
                    S.pool(lambda e: e.tensor_copy(out=qA[:, :, 0:64], in_=qa_f[:, :, 0:64]), [qa_f], [qA.k("n")])
                    S.pool(lambda e, t=t: e.tensor_tensor(out=tmr[:, 0, :, :], in0=qa_f[:, :, 64:96],
                                                          in1=cs_t[:, t:t + 1, :].to_broadcast([128, 6, 32]),
                                                          op=ALU.mult), [qa_f, cs_t], [tmr.k(0)])
                    S.pool(lambda e, t=t: e.tensor_tensor(out=tmr[:, 1, :, :], in0=qa_f[:, :, 96:128],
                                                          in1=sg_t[:, t:t + 1, :].to_broadcast([128, 6, 32]),
                                                          op=ALU.mult), [qa_f, sg_t], [tmr.k(1)])
                    S.pool(lambda e: e.tensor_tensor(out=qA[:, :, 64:96], in0=tmr[:, 0, :, :], in1=tmr[:, 1, :, :],
                                                     op=ALU.add), [tmr], [qA.k("r")])
                    for cg in range(2):
                        b = 5 + cg
                        for k in range(2):
                            mm(bank(b)[:, 0:384], cT[:, 6 + k, :], wukv_sb[:, k, cg * 384:(cg + 1) * 384], k == 0,
                               k == 1, [cT, wukv_sb], [pbuf[b]])
                        dst = kA[:, :, 0:64] if cg == 0 else Va_st[:, i, :, 0:64]
                        dw = [kA.k("n")] if cg == 0 else [Va_st.k(i)]
                        S.act(lambda e, b=b, dst=dst: e.activation(
                            out=dst, in_=bank(b)[:, 0:384].rearrange("p (a b) -> p a b", a=6, b=64),
                            func=AF.Identity, scale=rr[:, 1:2]), [pbuf[b], rr], dw)
                    S.pool(lambda e: e.tensor_copy(out=kA[:, :, 64:96],
                                                   in_=krope.ap.unsqueeze(1).to_broadcast([128, 6, 32])),
                           [krope], [kA.k("r")])
                    if PSTOP <= 4:
                        continue
                    tgroup([qA[:, h, :] for h in range(6)], [qA], 96, QaT_st[:, :, tsl], [QaT_st.k(i)], "act")
                    tgroup([kA[:, h, :] for h in range(6)], [kA], 96, KaT_st[:, :, tsl], [KaT_st.k(i)], "dve")
                    tgroup([qb_bf[:, h * 64:(h + 1) * 64] for h in range(6)], [qb_bf], 64, QbT_st[:, :, tsl],
                           [QbT_st.k(i)], "act")
                    tgroup([kb_bf[:, h * 64:(h + 1) * 64] for h in range(6)], [kb_bf], 64, KbT_st[:, :, tsl],
                           [KbT_st.k(i)], "dve")
                    tgroup([qc_bf[:, h * 64:(h + 1) * 64] for h in range(4)], [qc_bf], 64, QcT_st[:, :, tsl],
                           [QcT_st.k(i)], "act")
                    tgroup([kc_bf[:, h * 64:(h + 1) * 64] for h in range(4)], [kc_bf], 64, KcT_st[:, :, tsl],
                           [KcT_st.k(i)], "dve")
                    tgroup([qi_bf[:, 2 * j:2 * j + 2, :].rearrange("p a b -> p (a b)") for j in range(4)], [qi_bf], 128,
                           QiT_st[:, :, tsl], [QiT_st.k(i)], "act")
                    tgroup([ki_dup.ap.rearrange("p a b -> p (a b)")], [ki_dup], 128, KiT_st[:, tsl], [KiT_st.k(i)],
                           "act")
                    tgroup([gate_bf[:, c * 128:(c + 1) * 128] for c in range(8)], [gate_bf], 128, gT_st[:, :, tsl],
                           [gT_st.k(i)], "dve")
                if PSTOP <= 5:
                    continue
                gs = slice(g * 512, (g + 1) * 512)
                rows = slice(g * 512, (g + 1) * 512)
                for st_, dd, nm in [(QaT_st, QaT_d, "QaT"), (KaT_st, KaT_d, "KaT"), (QbT_st, QbT_d, "QbT"),
                                    (KbT_st, KbT_d, "KbT"), (QcT_st, QcT_d, "QcT"), (KcT_st, KcT_d, "KcT"),
                                    (QiT_st, QiT_d, "QiT"), (gT_st, gT_d, "gT")]:
                    S.dma(dd[:, :, gs], st_.ap, reads=[st_], writes=[d_bufs[nm].__getitem__(g)])
                S.dma(KiT_d[:, gs], KiT_st.ap, reads=[KiT_st], writes=[d_bufs["KiT"][g]])
                for st_, dd, nm, hh in [(Va_st, Va_d, "Va", 6), (Vb_st, Vb_d, "Vb", 6), (Vc_st, Vc_d, "Vc", 4)]:
                    S.dma(dd[rows, :].rearrange("(t p) c -> p t c", p=128),
                          st_.ap.rearrange("p t h c -> p t (h c)"), reads=[st_], writes=[d_bufs[nm][g]])
                S.dma(sgn_d[rows, :].rearrange("(t p) c -> p t c", p=128), sgn_st.ap, reads=[sgn_st],
                      writes=[d_bufs["sgn"][g]])
            S.barrier()
            sb.release(m0)

        def attend_dense(KT_d, QT_d, V_d, nmK, nmQ, nmV, H, Dk, aug_base, kind, yoff):
            m0 = sb.mark()
            DK = Dk + (32 if aug_base is not None else 0)
            Vall = sb.alloc([128, NT, H * 65], BF16, "Vall")
            for t8 in range(8):
                S.dma(Vall[:, t8 * 4:(t8 + 1) * 4, :],
                      V_d[t8 * 512:(t8 + 1) * 512, :].rearrange("(t p) c -> p t c", p=128),
                      reads=[d_bufs[nmV]], writes=[Vall.k(t8)])
            KT = [sb.alloc([DK, S_LEN], BF16, f"KT{i}") for i in range(2)]
            QT = [sb.alloc([DK, S_LEN], BF16, f"QT{i}") for i in range(2)]
            PT = [sb.alloc([128, 512], BF16, f"PT{i}") for i in range(3)]
            gTt = [sb.alloc([64, 512], BF16, f"gTt{i}") for i in range(2)]
            num_sb = sb.alloc([64, 512], F32, "num_sb")
            rden = sb.alloc([65, 512], F32, "rden")
            y1 = sb.alloc([64, 512], F32, "y1")
            yst = [sb.alloc([64, 512], BF16, f"yst{i}") for i in range(2)]
            rcnt = 0
            gcnt = 0
            for h in range(H):
                kt_, qt_ = KT[h % 2], QT[h % 2]
                S.dma(kt_[0:Dk, :], KT_d[:, h, :], reads=[d_bufs[nmK]], writes=[kt_.k("d")])
                S.dma(qt_[0:Dk, :], QT_d[:, h, :], reads=[d_bufs[nmQ]], writes=[qt_.k("d")])
                if aug_base is not None:
                    S.dma(kt_[Dk:DK, :], kpos_d[aug_base + h], writes=[kt_.k("a")])
                    S.dma(qt_[Dk:DK, :], qpos_d[aug_base + h], writes=[qt_.k("a")])
                for g in range(NG):
                    if kind == "causal":
                        kts = list(range(0, 4 * g + 4))
                    else:
                        kts = list(range(max(0, 4 * g - 16), 4 * g + 4))
                    acc_b = 3 + gcnt % 2
                    den_b = 5 + gcnt % 2
                    gt = gTt[gcnt % 2]
                    ys = yst[gcnt % 2]
                    row0 = yoff + h * 64
                    ch, r0 = row0 // 128, row0 % 128
                    S.dma(gt.ap, gT_d[r0:r0 + 64, ch, g * 512:(g + 1) * 512], reads=[d_bufs["gT"]], writes=[gt])
                    qsl = slice(g * 512, (g + 1) * 512)

                    def c0_of(kt):
                        return max(0, kt - 4 * g) * 128

                    def score(j, r):
                        kt = kts[j]
                        c0 = c0_of(kt)
                        mm(bank(r % 3)[:, c0:512], kt_[0:DK, kt * 128:(kt + 1) * 128],
                           qt_[0:DK, g * 512 + c0:(g + 1) * 512], True, True, [kt_, qt_], [pbuf[r % 3]])

                    LA = 2
                    for j in range(min(LA, len(kts))):
                        score(j, rcnt + j)
                    for j, kt in enumerate(kts):
                        r = rcnt + j
                        p_ = PT[r % 3]
                        c0 = c0_of(kt)
                        S.act(lambda e, r=r, p_=p_, c0=c0: e.activation(out=p_[:, c0:512], in_=bank(r % 3)[:, c0:512],
                                                                        func=AF.Exp), [pbuf[r % 3]], [p_])
                        j0 = 4 * g - kt
                        if kind == "dilated":
                            msl = mtab[:, (j0 + 3) * 128 + c0:(j0 + 7) * 128]
                            S.dve(lambda e, p_=p_, msl=msl, c0=c0: e.tensor_tensor(out=p_[:, c0:512], in0=p_[:, c0:512],
                                                                                  in1=msl, op=ALU.mult),
                                  [p_, mtab], [p_])
                        elif j0 < 1:
                            msl = ctab[:, (j0 + 3) * 128 + c0:(j0 + 7) * 128]
                            S.dve(lambda e, p_=p_, msl=msl, c0=c0: e.tensor_tensor(out=p_[:, c0:512], in0=p_[:, c0:512],
                                                                                  in1=msl, op=ALU.mult),
                                  [p_, ctab], [p_])
                        if j + LA < len(kts):
                            score(j + LA, r + LA)
                        last = (j == len(kts) - 1)
                        mm(bank(acc_b)[0:64, c0:512], Vall[:, kt, h * 65:h * 65 + 64], p_[:, c0:512], j == 0, last,
                           [Vall, p_], [pbuf[acc_b]], nr=([p_] if last else ()))
                        mm(bank(den_b)[0:64, c0:512], ones_b.ap, p_[:, c0:512], j == 0, last,
                           [ones_b, p_], [pbuf[den_b]], nr=([p_] if last else ()))
                        p_last = p_
                    rcnt += len(kts)
                    S.dve(lambda e, den_b=den_b: e.reciprocal(out=num_sb.ap, in_=bank(den_b)[0:64, :]),
                          [pbuf[den_b], p_last], [num_sb])
                    S.dve(lambda e, acc_b=acc_b: e.tensor_tensor(out=y1.ap, in0=bank(acc_b)[0:64, :], in1=num_sb.ap,
                                                                 op=ALU.mult), [pbuf[acc_b], num_sb], [y1])
                    S.dve(lambda e, ys=ys, gt=gt: e.tensor_tensor(out=ys.ap, in0=y1.ap, in1=gt.ap, op=ALU.mult),
                          [y1, gt], [ys])
                    S.dma(yT_d[row0:row0 + 64, qsl], ys.ap, reads=[ys], writes=[d_bufs["yT"][(row0, g)]])
                    gcnt += 1
            S.barrier()
            sb.release(m0)

        if "A" in phases:
            attend_dense(KaT_d, QaT_d, Va_d, "KaT", "QaT", "Va", 6, 96, None, "causal", 0)
        if "B" in phases:
            attend_dense(KbT_d, QbT_d, Vb_d, "KbT", "QbT", "Vb", 6, 64, 0, "dilated", 384)

        if "C" in phases:
            m0 = sb.mark()
            KTc = sb.alloc([96, 4, S_LEN], BF16, "KTc")
            S.dma(KTc[0:64, :, :], KcT_d, reads=[d_bufs["KcT"]], writes=[KTc.k("d")])
            for h in range(4):
                S.dma(KTc[64:96, h, :], kpos_d[6 + h], writes=[KTc.k(("a", h))])
            Vc = sb.alloc([128, NT, 260], BF16, "Vc")
            for t8 in range(8):
                S.dma(Vc[:, t8 * 4:(t8 + 1) * 4, :],
                      Vc_d[t8 * 512:(t8 + 1) * 512, :].rearrange("(t p) c -> p t c", p=128),
                      reads=[d_bufs["Vc"]], writes=[Vc.k(t8)])
            KiT = sb.alloc([128, S_LEN], BF16, "KiT")
            S.dma(KiT.ap, KiT_d, reads=[d_bufs["KiT"]], writes=[KiT])
            QTc = [sb.alloc([96, 4, 128], BF16, f"QTc{i}") for i in range(2)]
            QiT = [sb.alloc([128, 4, 128], BF16, f"QiT{i}") for i in range(2)]
            sgn = [sb.alloc([128, 8], F32, f"sgn{i}") for i in range(2)]
            gTc = [sb.alloc([64, 4, 128], BF16, f"gTc{i}") for i in range(2)]
            score = sb.alloc([128, S_LEN], F32, "score")
            junkb = sb.alloc([128, S_LEN], BF16, "junkb")
            maskb = sb.alloc([128, S_LEN], BF16, "maskb")
            mT = sb.alloc([128, S_LEN], BF16, "mT")
            rl = [sb.alloc([128, 512], BF16, f"rl{i}") for i in range(3)]
            PTc = [sb.alloc([128, 512], BF16, f"PTc{i}") for i in range(2)]
            m8 = sb.alloc([128, 8], F32, "m8")
            lo = sb.alloc([128, 1], F32, "lo")
            w0 = sb.alloc([128, 1], F32, "w0")
            whalf = sb.alloc([128, 16], F32, "whalf")
            mid = sb.alloc([128, 1], F32, "mid")
            cnt = sb.alloc([128, 1], F32, "cnt")
            ssp = sb.alloc([128, 1], F32, "ssp")
            junka = sb.alloc([128, S_LEN // 2 + 128], BF16, "junka")
            stp = sb.alloc([128, 1], F32, "stp")
            numc = sb.alloc([64, 128], F32, "numc")
            rdenc = sb.alloc([65, 128], F32, "rdenc")
            y1c = sb.alloc([64, 128], F32, "y1c")
            ystc = [sb.alloc([64, 4, 128], BF16, f"ystc{i}") for i in range(2)]
            NIT = 14
            ir = 0
            sr = 0
            for qt in range(NT):
                n = qt + 1
                N = 128 * n
                qsl = slice(qt * 128, (qt + 1) * 128)
                Q_ = QTc[qt % 2]; Qi_ = QiT[qt % 2]; sg_ = sgn[qt % 2]; gt = gTc[qt % 2]; ys = ystc[qt % 2]
                S.dma(Q_[0:64, :, :], QcT_d[:, :, qsl], reads=[d_bufs["QcT"]], writes=[Q_.k("d")])
                for h in range(4):
                    S.dma(Q_[64:96, h, :], qpos_d[6 + h][:, qsl], writes=[Q_.k(("a", h))])
                S.dma(Qi_.ap, QiT_d[:, :, qsl], reads=[d_bufs["QiT"]], writes=[Qi_])
                S.dma(sg_.ap, sgn_d[qsl, :], reads=[d_bufs["sgn"]], writes=[sg_])
                for c2 in range(2):
                    S.dma(gt[:, 2 * c2:2 * c2 + 2, :], gT_d[:, 6 + c2, qsl].rearrange("(a p) t -> p a t", p=64),
                          reads=[d_bufs["gT"]], writes=[gt.k(c2)])
                nch = (N + 511) // 512
                for c in range(nch):
                    w = min(512, N - 512 * c)
                    csl = slice(c * 512, c * 512 + w)
                    for h in range(8):
                        b = ir % 3
                        pr = (h % 2) * 64
                        mm(bank(b)[:, 0:w], Qi_[pr:pr + 64, h // 2, :], KiT[pr:pr + 64, csl], True, True,
                           [Qi_, KiT], [pbuf[b]])
                        r_ = rl[ir % 3]
                        S.act(lambda e, b=b, w=w, r_=r_: e.activation(out=r_[:, 0:w], in_=bank(b)[:, 0:w], func=AF.Relu),
                              [pbuf[b]], [r_])
                        if h == 0:
                            S.dve(lambda e, r_=r_, w=w, csl=csl, sg_=sg_: e.tensor_scalar(
                                out=score[:, csl], in0=r_[:, 0:w], scalar1=sg_[:, 0:1], scalar2=None, op0=ALU.mult),
                                [r_, sg_], [score.k(c)])
                        else:
                            S.dve(lambda e, r_=r_, w=w, csl=csl, sg_=sg_, h=h: e.scalar_tensor_tensor(
                                out=score[:, csl], in0=r_[:, 0:w], scalar=sg_[:, h:h + 1], in1=score[:, csl],
                                op0=ALU.mult, op1=ALU.add), [r_, sg_, score.k(c)], [score.k(c)])
                        ir += 1
                thr = lo
                if qt >= 2:
                    S.dve(lambda e, N=N: e.tensor_reduce(out=lo.ap, in_=score[:, 0:N], axis=AX.X, op=ALU.min),
                          [score], [lo])
                S.dve(lambda e, N=N: e.tensor_tensor(out=score[:, N - 128:N], in0=score[:, N - 128:N], in1=negm.ap,
                                                     op=ALU.add), [score, negm], [score])
                if qt >= 2:
                    S.dve(lambda e, N=N: e.max(out=m8.ap, in_=score[:, 0:N]), [score], [m8])
                    S.dve(lambda e: e.tensor_tensor(out=w0.ap, in0=m8[:, 0:1], in1=lo.ap, op=ALU.subtract),
                          [m8, lo], [w0])
                    S.dve(lambda e: e.tensor_scalar(out=whalf.ap, in0=halfpow.ap, scalar1=w0[:, 0:1], scalar2=None,
                                                    op0=ALU.mult), [halfpow, w0], [whalf])
                    S.dve(lambda e: e.tensor_tensor(out=mid.ap, in0=lo.ap, in1=whalf[:, 0:1], op=ALU.add),
                          [lo, whalf], [mid])
                    N1 = ((n + 1) // 2) * 128
                    N2 = N - N1
                    thrN = 256.0 - N2 / 2.0
                    for it in range(NIT):
                        S.act(lambda e, N1=N1, N=N, N2=N2: e.activation(out=junka[:, 0:N2], in_=score[:, N1:N],
                                                                       func=AF.Sign, scale=-1.0, bias=mid[:, 0:1],
                                                                       accum_out=ssp[:, 0:1]),
                              [score, mid], [junka, ssp])
                        S.dve(lambda e, N1=N1: e.tensor_scalar(out=junkb[:, 0:N1], in0=score[:, 0:N1],
                                                               scalar1=mid[:, 0:1], scalar2=None, op0=ALU.is_ge,
                                                               op1=ALU.add, accum_out=cnt[:, 0:1]),
                              [score, mid], [junkb, cnt])
                        S.dve(lambda e: e.scalar_tensor_tensor(out=stp.ap, in0=ssp.ap, scalar=-0.5, in1=cnt.ap,
                                                               op0=ALU.mult, op1=ALU.add), [ssp, cnt], [stp])
                        lastit = (it == NIT - 1)
                        S.dve(lambda e, lastit=lastit, thrN=thrN: e.tensor_scalar(
                            out=stp.ap, in0=stp.ap, scalar1=float(thrN), scalar2=(-1.0 if lastit else -0.5),
                            op0=ALU.is_ge, op1=ALU.add), [stp], [stp])
                        dst = lo if lastit else mid
                        S.dve(lambda e, it=it, dst=dst: e.scalar_tensor_tensor(
                            out=dst.ap, in0=stp.ap, scalar=whalf[:, it:it + 1], in1=mid.ap, op0=ALU.mult,
                            op1=ALU.add), [stp, whalf, mid], [dst])
                else:
                    S.dve(lambda e: e.memset(lo.ap, -1e29), [], [lo])
                S.dve(lambda e, N=N: e.tensor_scalar(out=maskb[:, 0:N], in0=score[:, 0:N], scalar1=thr[:, 0:1],
                                                     scalar2=None, op0=ALU.is_ge), [score, lo], [maskb])
                nkb = (n + 3) // 4
                for kb in range(nkb):
                    nb = min(4, n - 4 * kb)
                    pv = bank_bf(7)
                    for j in range(nb):
                        kt = 4 * kb + j
                        tr(pv[:, j * 128:(j + 1) * 128], maskb[:, kt * 128:(kt + 1) * 128], ident_b.ap,
                           [maskb, ident_b], [pbuf[7]])
                    S.act(lambda e, pv=pv, kb=kb, nb=nb: e.activation(out=mT[:, kb * 512:kb * 512 + nb * 128],
                                                                      in_=pv[:, 0:nb * 128], func=AF.Copy),
                          [pbuf[7]], [mT.k(kb)])
                for h in range(4):
                    for kb in range(nkb):
                        nb = min(4, n - 4 * kb)
                        b = 3 + sr % 2
                        p_ = PTc[sr % 2]
                        for j in range(nb):
                            kt = 4 * kb + j
                            mm(bank(b)[:, j * 128:(j + 1) * 128], KTc[0:96, h, kt * 128:(kt + 1) * 128], Q_[0:96, h, :],
                               True, True, [KTc, Q_], [pbuf[b]])
                        S.act(lambda e, b=b, nb=nb, p_=p_: e.activation(out=p_[:, 0:nb * 128], in_=bank(b)[:, 0:nb * 128],
                                                                        func=AF.Exp), [pbuf[b]], [p_])
                        S.dve(lambda e, p_=p_, nb=nb, kb=kb: e.tensor_tensor(
                            out=p_[:, 0:nb * 128], in0=p_[:, 0:nb * 128], in1=mT[:, kb * 512:kb * 512 + nb * 128],
                            op=ALU.mult), [p_, mT.k(kb)], [p_])
                        for j in range(nb):
                            kt = 4 * kb + j
                            mm(bank(5)[0:64, 0:128], Vc[:, kt, h * 65:h * 65 + 64], p_[:, j * 128:(j + 1) * 128],
                               kt == 0, kt == n - 1, [Vc, p_], [pbuf[5]], nr=([p_] if kb == nkb - 1 else ()))
                            mm(bank(6)[0:64, 0:128], ones_b.ap, p_[:, j * 128:(j + 1) * 128],
                               kt == 0, kt == n - 1, [ones_b, p_], [pbuf[6]], nr=([p_] if kb == nkb - 1 else ()))
                            p_last = p_
                        sr += 1
                    S.dve(lambda e: e.reciprocal(out=numc.ap, in_=bank(6)[0:64, 0:128]), [pbuf[6], p_last], [numc])
                    S.dve(lambda e: e.tensor_tensor(out=y1c.ap, in0=bank(5)[0:64, 0:128], in1=numc.ap, op=ALU.mult),
                          [pbuf[5], numc], [y1c])
                    S.dve(lambda e, ys=ys, gt=gt, h=h: e.tensor_tensor(out=ys[:, h, :], in0=y1c.ap, in1=gt[:, h, :],
                                                                       op=ALU.mult), [y1c, gt], [ys.k(h)])
                S.dma(yT_d[768:1024, qsl].rearrange("(h p) t -> p h t", p=64), ys.ap, reads=[ys],
                      writes=[d_bufs["yT"][("c", qt)]])
            S.barrier()
            sb.release(m0)

        if "O" in phases:
            m0 = sb.mark()
            w_out_sb = sb.alloc([128, 8, DM], BF16, "w_out")
            for k in range(8):
                S.dma(w_out_sb[:, k, :], w_out[l, k * 128:(k + 1) * 128, :], writes=[w_out_sb.k(k)], q="pool")
            lng = sb.alloc([128, DM], F32, "lng")
            lnb = sb.alloc([128, DM], F32, "lnb")
            S.dma(lng.ap, lng_b[l], writes=[lng])
            S.dma(lnb.ap, lnb_b[l], writes=[lnb])
            yTs = [sb.alloc([128, 8, 512], BF16, f"yTs{i}") for i in range(2)]
            xo = [sb.alloc([128, DM], F32, f"xo{i}") for i in range(2)]
            zt = [sb.alloc([128, DM], F32, f"zt{i}") for i in range(2)]
            ot = [sb.alloc([128, DM], F32, f"ot{i}") for i in range(2)]
            stats = sb.alloc([128, 2, 6], F32, "stats")
            mv = sb.alloc([128, 2], F32, "mv")
            rs = sb.alloc([128, 1], F32, "rs")
            for g in range(NG):
                yt_ = yTs[g % 2]
                S.dma(yt_.ap, yT_d[:, g * 512:(g + 1) * 512].rearrange("(c p) t -> p c t", p=128),
                      reads=[d_bufs["yT"]], writes=[yt_])
                for i in range(4):
                    t = 4 * g + i
                    x_ = xo[t % 2]; z_ = zt[t % 2]; o_ = ot[t % 2]
                    S.dma(x_.ap, x_src[t * 128:(t + 1) * 128, :], reads=xr, writes=[x_])
                    for cg in range(2):
                        b = (2 * t + cg) % 4
                        for k in range(8):
                            mm(bank(b), yt_[:, k, i * 128:(i + 1) * 128], w_out_sb[:, k, cg * 512:(cg + 1) * 512],
                               k == 0, k == 7, [yt_, w_out_sb], [pbuf[b]], nr=[yt_])
                        csl = slice(cg * 512, (cg + 1) * 512)
                        S.dve(lambda e, b=b, z_=z_, csl=csl: e.tensor_tensor(out=z_[:, csl], in0=bank(b),
                                                                            in1=gate_b[:, csl], op=ALU.mult),
                              [pbuf[b], gate_b, yt_], [z_.k(cg)])
                        S.dve(lambda e, z_=z_, x_=x_, csl=csl: e.scalar_tensor_tensor(
                            out=z_[:, csl], in0=x_[:, csl], scalar=float(ALPHA), in1=z_[:, csl], op0=ALU.mult,
                            op1=ALU.add), [x_, z_.k(cg)], [z_.k(cg)])
                        S.dve(lambda e, z_=z_, csl=csl, cg=cg: e.bn_stats(out=stats[:, cg, :], in_=z_[:, csl]),
                              [z_.k(cg)], [stats.k(cg)])
                    S.dve(lambda e: e.bn_aggr(out=mv.ap, in_=stats.ap.rearrange("p a b -> p (a b)")), [stats], [mv])
                    S.dve(lambda e: e.tensor_scalar(out=rs.ap, in0=mv[:, 1:2], scalar1=1e-5, scalar2=None, op0=ALU.add),
                          [mv], [rs])
                    S.act(lambda e: e.activation(out=rs.ap, in_=rs.ap, func=AF.Sqrt), [rs], [rs])
                    S.dve(lambda e: e.reciprocal(out=rs.ap, in_=rs.ap), [rs], [rs])
                    S.dve(lambda e, z_=z_: e.tensor_scalar(out=z_.ap, in0=z_.ap, scalar1=mv[:, 0:1], scalar2=rs[:, 0:1],
                                                           op0=ALU.subtract, op1=ALU.mult), [z_, mv, rs], [z_])
                    S.pool(lambda e, z_=z_, o_=o_: e.tensor_tensor(out=o_.ap, in0=z_.ap, in1=lng.ap, op=ALU.mult),
                           [z_, lng], [o_])
                    S.pool(lambda e, o_=o_: e.tensor_tensor(out=o_.ap, in0=o_.ap, in1=lnb.ap, op=ALU.add),
                           [o_, lnb], [o_])
                    S.dma(x_dst[t * 128:(t + 1) * 128, :], o_.ap, reads=[o_], writes=[x_dst_buf[t]])
            S.barrier()
            sb.release(m0)

    S.barrier()
    info = S.emit()
    return nc, info


def _consts():
    bf = ml_dtypes.bfloat16
    cst = {}
    cst["ident_f"] = np.eye(128, dtype=np.float32)
    cst["ident_b"] = np.eye(128, dtype=np.float32).astype(bf)
    pos = np.arange(S_LEN, dtype=np.float32)
    freqs = (10000.0 ** (-np.arange(0, 32, 2, dtype=np.float32) / 32)).astype(np.float32)
    ang = (pos[:, None] * freqs[None, :]).astype(np.float32)
    cos, sin = np.cos(ang).astype(np.float32), np.sin(ang).astype(np.float32)
    cs = np.concatenate([cos, cos], 1)
    sg = np.concatenate([-sin, sin], 1)
    cst["cs_t"] = np.ascontiguousarray(cs.reshape(NT, 128, 32).transpose(1, 0, 2))
    cst["sg_t"] = np.ascontiguousarray(sg.reshape(NT, 128, 32).transpose(1, 0, 2))
    ki = np.arange(128)[:, None]
    qi = np.arange(128)[None, :]
    mt = np.zeros((128, 23, 128), np.float32)
    for jj in range(23):
        j = jj - 3
        d = 128 * j + qi - ki
        m = ((d >= 0) & (d <= 128)).astype(np.float32) + ((d >= 0) & (d % 4 == 0) & (d <= 512)) \
            + ((d >= 0) & (d % 16 == 0) & (d <= 2048))
        mt[:, jj, :] = m
    cst["mtab"] = mt.reshape(128, 23 * 128).astype(bf)
    ct = np.zeros((128, 7, 128), np.float32)
    for jj in range(7):
        j = jj - 3
        d = 128 * j + qi - ki
        ct[:, jj, :] = (d >= 0)
    cst["ctab"] = ct.reshape(128, 7 * 128).astype(bf)
    cst["negm"] = np.where(np.arange(128)[None, :] <= np.arange(128)[:, None], 0.0, -1e30).astype(np.float32)
    cst["halfpow"] = np.tile((0.5 ** np.arange(1, 17)).astype(np.float32)[None, :], (128, 1))
    kpos = np.zeros((N_ALIBI, 32, S_LEN), np.float32)
    qpos = np.zeros((N_ALIBI, 32, S_LEN), np.float32)
    tok = np.arange(S_LEN)
    u = (tok % 128 - 64).astype(np.float32)
    tt = (tok // 128).astype(np.float32)
    for h in range(N_ALIBI):
        c1 = np.float32(np.float32(SLOPES[h]).astype(bf))
        c2 = np.float32(np.float32(np.float32(SLOPES[h]) - c1).astype(bf))
        kpos[h, 0:6] = np.stack([u, u, tt, tt, np.full(S_LEN, c1), np.full(S_LEN, c2)])
        qpos[h, 0:6] = np.stack([np.full(S_LEN, c1), np.full(S_LEN, c2), np.full(S_LEN, 128 * c1),
                                 np.full(S_LEN, 128 * c2), -128 * tt, -128 * tt])
    cst["kpos"] = kpos.astype(bf)
    cst["qpos"] = qpos.astype(bf)
    return cst


def _prep_shared(w_ada, b_ada, w_in, q_norm_g, kv_norm_g, w_uq, w_uk, w_uv, w_out, ln_g, ln_b):
    L = w_in.shape[0]
    sh = {}
    sh["w_ada"] = np.ascontiguousarray(w_ada, dtype=np.float32)
    sh["b_ada"] = np.ascontiguousarray(b_ada, dtype=np.float32).reshape(L, 1, 3 * DM)
    kr = w_in[:, :, 1024:1056]
    krs = np.concatenate([kr[:, :, 16:32], kr[:, :, 0:16]], axis=2)
    sh["w_in"] = np.ascontiguousarray(np.concatenate([w_in[:, :, :1056], krs, w_in[:, :, 1056:]], axis=2),
                                      dtype=np.float32)
    uq = w_uq.reshape(L, 768, 6, 96)
    rope = uq[..., 64:96]
    rope_s = np.concatenate([rope[..., 16:32], rope[..., 0:16]], axis=-1)
    sh["w_uq"] = np.ascontiguousarray(np.concatenate([uq, rope_s], axis=-1).reshape(L, 768, 768), dtype=np.float32)
    sh["w_ukv"] = np.ascontiguousarray(np.concatenate([w_uk, w_uv], axis=2), dtype=np.float32)
    sh["w_out"] = np.ascontiguousarray(w_out, dtype=np.float32)
    sh["qg_col"] = np.ascontiguousarray(q_norm_g.reshape(L, 6, 128).transpose(0, 2, 1), dtype=np.float32)
    sh["kvg_col"] = np.ascontiguousarray(kv_norm_g.reshape(L, 2, 128).transpose(0, 2, 1), dtype=np.float32)
    sh["lng_b"] = np.ascontiguousarray(np.broadcast_to(ln_g[:, None, :], (L, 128, DM)), dtype=np.float32)
    sh["lnb_b"] = np.ascontiguousarray(np.broadcast_to(ln_b[:, None, :], (L, 128, DM)), dtype=np.float32)
    return sh


_NC_CACHE = {}


def kernel(x, c, w_ada, b_ada, w_in, q_norm_g, kv_norm_g, w_uq, w_uk, w_uv, w_out, ln_g, ln_b):
    x = np.asarray(x, dtype=np.float32)
    c = np.asarray(c, dtype=np.float32)
    args = [np.asarray(a, dtype=np.float32) for a in
            (w_ada, b_ada, w_in, q_norm_g, kv_norm_g, w_uq, w_uk, w_uv, w_out, ln_g, ln_b)]
    shared = _prep_shared(*args)
    shared.update(_consts())
    if "nc" not in _NC_CACHE:
        _NC_CACHE["nc"] = build_nc()[0]
    nc = _NC_CACHE["nc"]
    B = x.shape[0]
    in_maps = []
    for b in range(B):
        m = dict(shared)
        m["x"] = np.ascontiguousarray(x[b])
        m["c_col"] = np.ascontiguousarray(c[b].reshape(8, 128).T)
        in_maps.append(m)
    res = run_bass_kernel_spmd(nc, in_maps, core_ids=list(range(B)))
    return np.stack([np.asarray(r["out"], dtype=np.float32) for r in res.results], axis=0)
```
